# Optimizing a Trainium2 kernel written in Bass

```python
import math
import jax, jax.numpy as jnp
from jax import lax
import numpy as np

D_MODEL = 1024
BATCH = 8
SEQ = 4096
DEPTH = 1

SB_HEADS = 8
SB_HEAD_DIM = 64
SB_WIDTH = SB_HEADS * SB_HEAD_DIM
DIFF_HEADS = 4
DIFF_HEAD_DIM = 64
DIFF_V_DIM = 2 * DIFF_HEAD_DIM
DIFF_QK_WIDTH = DIFF_HEADS * 2 * DIFF_HEAD_DIM
DIFF_V_WIDTH = DIFF_HEADS * DIFF_V_DIM
N_BRANCHES = 2
IN_WIDTH = 3 * SB_WIDTH + 2 * DIFF_QK_WIDTH + DIFF_V_WIDTH + N_BRANCHES * D_MODEL
Q_BLOCK = 128
N_EXPERTS = 32
TOP_K = 4
D_EXPERT = D_MODEL
SWIGLU_LIMIT = 7.0
SWIGLU_ALPHA = 1.702
RMS_EPS = 1e-6
N_MOD = 6

kernel_name = 'hybrid_sb_diffattn_moe_block'


def rms_norm(x, g):
    xf = x.astype(jnp.float32)
    y = xf * lax.rsqrt(jnp.mean(xf * xf, axis=-1, keepdims=True) + RMS_EPS)
    return (y * g.astype(jnp.float32)).astype(x.dtype)


def modulate(h, shift, scale):
    return h * (1.0 + scale[:, None, :]) + shift[:, None, :]


def alibi_slopes(n_heads):
    return 2.0 ** (-8.0 * jnp.arange(1, n_heads + 1, dtype=jnp.float32) / n_heads)


def _q_blocks(t):
    b, s, h, d = t.shape
    return t.reshape(b, s // Q_BLOCK, Q_BLOCK, h, d).transpose(1, 0, 3, 2, 4)


def _merge_blocks(o):
    nb, b, h, q, d = o.shape
    return o.transpose(1, 0, 3, 2, 4).reshape(b, nb * q, h, d)


def stick_breaking_attention(q, k, v):
    b, s, h, d = q.shape
    kh = k.transpose(0, 2, 1, 3)
    vh = v.transpose(0, 2, 1, 3)
    kpos = jnp.arange(s)
    scale = 1.0 / math.sqrt(SB_HEAD_DIM)

    def block(args):
        qb, t0 = args
        z = jnp.einsum('bhqd,bhkd->bhqk', qb, kh).astype(jnp.float32) * scale
        qpos = t0 + jnp.arange(Q_BLOCK)
        mask = kpos[None, :] < qpos[:, None]
        log_1mb = jnp.where(mask, jax.nn.log_sigmoid(-z), 0.0)
        between = lax.cumsum(log_1mb, axis=3, reverse=True) - log_1mb
        a = jnp.where(mask, jnp.exp(jax.nn.log_sigmoid(z) + between), 0.0)
        return jnp.einsum('bhqk,bhkd->bhqd', a.astype(vh.dtype), vh)

    starts = jnp.arange(s // Q_BLOCK) * Q_BLOCK
    o = _merge_blocks(lax.map(block, (_q_blocks(q), starts)))
    return o.reshape(b, s, h * d)


def differential_attention(q1, q2, k1, k2, v, lam, lam_init, g_subln):
    b, s, h, d = q1.shape
    k1h = k1.transpose(0, 2, 1, 3)
    k2h = k2.transpose(0, 2, 1, 3)
    vh = v.transpose(0, 2, 1, 3)
    kpos = jnp.arange(s)
    slopes = alibi_slopes(DIFF_HEADS)
    scale = 1.0 / math.sqrt(DIFF_HEAD_DIM)

    def block(args):
        q1b, q2b, t0 = args
        qpos = t0 + jnp.arange(Q_BLOCK)
        dist = (qpos[:, None] - kpos[None, :]).astype(jnp.float32)
        mask = dist >= 0.0
        bias = -slopes[:, None, None] * dist

        def probs(qb, kb):
            z = jnp.einsum('bhqd,bhkd->bhqk', qb, kb).astype(jnp.float32) * scale + bias
            return jax.nn.softmax(jnp.where(mask, z, -jnp.inf), axis=-1)

        w = probs(q1b, k1h) - lam * probs(q2b, k2h)
        return jnp.einsum('bhqk,bhkv->bhqv', w.astype(vh.dtype), vh)

    starts = jnp.arange(s // Q_BLOCK) * Q_BLOCK
    o = _merge_blocks(lax.map(block, (_q_blocks(q1), _q_blocks(q2), starts)))
    o = rms_norm(o, g_subln) * (1.0 - lam_init)
    return o.reshape(b, s, h * DIFF_V_DIM)


def moe_ffn(h, w_router, b_router, w_gate_up, b_gate_up, w_down, b_down):
    b, s, d = h.shape
    hf = h.reshape(b * s, d)
    logits = (hf @ w_router + b_router).astype(jnp.float32)
    top_val, top_idx = lax.top_k(logits, TOP_K)
    top_w = jax.nn.softmax(top_val, axis=-1)
    combine = jnp.sum(jax.nn.one_hot(top_idx, N_EXPERTS, dtype=jnp.float32) * top_w[..., None],
                      axis=1).astype(h.dtype)
    y = jnp.zeros_like(hf)
    for e in range(N_EXPERTS):
        gu = hf @ w_gate_up[e] + b_gate_up[e]
        gate = jnp.minimum(gu[:, :D_EXPERT], SWIGLU_LIMIT)
        up = jnp.clip(gu[:, D_EXPERT:], -SWIGLU_LIMIT, SWIGLU_LIMIT)
        act = (up + 1.0) * (gate * jax.nn.sigmoid(SWIGLU_ALPHA * gate))
        y = y + combine[:, e:e + 1] * (act @ w_down[e] + b_down[e])
    return y.reshape(b, s, d)


def setup_inputs(seed: int = 0) -> dict:
    key = jax.random.key(seed)
    ks = jax.random.split(key, 24)
    D = D_MODEL
    L = DEPTH

    def nrm(k, shape, std):
        return jax.random.normal(k, shape, jnp.float32) * std

    return {
        'x': nrm(ks[0], (BATCH, SEQ, D), 1.0),
        'c': nrm(ks[1], (BATCH, D), 1.0),
        'w_mod': nrm(ks[2], (L, D, N_MOD * D), 0.5 * D ** -0.5),
        'b_mod': nrm(ks[3], (L, N_MOD * D), 0.01),
        'g_pre_mix': 1.0 + nrm(ks[4], (L, D), 0.05),
        'g_post_mix': 1.0 + nrm(ks[5], (L, D), 0.05),
        'w_in': nrm(ks[6], (L, D, IN_WIDTH), D ** -0.5),
        'lambda_q1': nrm(ks[7], (L, DIFF_HEAD_DIM), 0.1),
        'lambda_k1': nrm(ks[8], (L, DIFF_HEAD_DIM), 0.1),
        'lambda_q2': nrm(ks[9], (L, DIFF_HEAD_DIM), 0.1),
        'lambda_k2': nrm(ks[10], (L, DIFF_HEAD_DIM), 0.1),
        'g_subln': 1.0 + nrm(ks[11], (L, DIFF_V_DIM), 0.05),
        'w_branch_sb': nrm(ks[12], (L, SB_WIDTH, D), SB_WIDTH ** -0.5),
        'w_branch_diff': nrm(ks[13], (L, DIFF_V_WIDTH, D), DIFF_V_WIDTH ** -0.5),
        'w_out': nrm(ks[14], (L, D, D), D ** -0.5),
        'g_pre_ffn': 1.0 + nrm(ks[15], (L, D), 0.05),
        'g_post_ffn': 1.0 + nrm(ks[16], (L, D), 0.05),
        'w_router': nrm(ks[17], (L, D, N_EXPERTS), D ** -0.5),
        'b_router': nrm(ks[18], (L, N_EXPERTS), 0.01),
        'w_gate_up': nrm(ks[19], (L, N_EXPERTS, D, 2 * D_EXPERT), D ** -0.5),
        'b_gate_up': nrm(ks[20], (L, N_EXPERTS, 2 * D_EXPERT), 0.01),
        'w_down': nrm(ks[21], (L, N_EXPERTS, D_EXPERT, D), D_EXPERT ** -0.5),
        'b_down': nrm(ks[22], (L, N_EXPERTS, D), 0.01),
    }


def reference(x, c, w_mod, b_mod, g_pre_mix, g_post_mix, w_in, lambda_q1, lambda_k1,
              lambda_q2, lambda_k2, g_subln, w_branch_sb, w_branch_diff, w_out,
              g_pre_ffn, g_post_ffn, w_router, b_router, w_gate_up, b_gate_up,
              w_down, b_down):
    b, s, d = x.shape
    c_act = jax.nn.silu(c)
    for l in range(DEPTH):
        mod = c_act @ w_mod[l] + b_mod[l]
        sh_m, sc_m, gt_m, sh_f, sc_f, gt_f = jnp.split(mod, N_MOD, axis=-1)

        h = modulate(rms_norm(x, g_pre_mix[l]), sh_m, sc_m)
        proj = h @ w_in[l]
        o0 = 0
        q_sb = proj[..., o0:o0 + SB_WIDTH].reshape(b, s, SB_HEADS, SB_HEAD_DIM); o0 += SB_WIDTH
        k_sb = proj[..., o0:o0 + SB_WIDTH].reshape(b, s, SB_HEADS, SB_HEAD_DIM); o0 += SB_WIDTH
        v_sb = proj[..., o0:o0 + SB_WIDTH].reshape(b, s, SB_HEADS, SB_HEAD_DIM); o0 += SB_WIDTH
        q_df = proj[..., o0:o0 + DIFF_QK_WIDTH].reshape(b, s, DIFF_HEADS, 2, DIFF_HEAD_DIM); o0 += DIFF_QK_WIDTH
        k_df = proj[..., o0:o0 + DIFF_QK_WIDTH].reshape(b, s, DIFF_HEADS, 2, DIFF_HEAD_DIM); o0 += DIFF_QK_WIDTH
        v_df = proj[..., o0:o0 + DIFF_V_WIDTH].reshape(b, s, DIFF_HEADS, DIFF_V_DIM); o0 += DIFF_V_WIDTH
        gates = jax.nn.sigmoid(proj[..., o0:o0 + N_BRANCHES * d])
        gate_sb, gate_df = gates[..., :d], gates[..., d:]

        y_sb = stick_breaking_attention(q_sb, k_sb, v_sb)
        lam_init = 0.8 - 0.6 * math.exp(-0.3 * l)
        lam = (jnp.exp(jnp.sum(lambda_q1[l].astype(jnp.float32) * lambda_k1[l].astype(jnp.float32)))
               - jnp.exp(jnp.sum(lambda_q2[l].astype(jnp.float32) * lambda_k2[l].astype(jnp.float32)))
               + lam_init)
        y_df = differential_attention(q_df[..., 0, :], q_df[..., 1, :], k_df[..., 0, :],
                                      k_df[..., 1, :], v_df, lam, lam_init, g_subln[l])

        merged = gate_sb * (y_sb @ w_branch_sb[l]) + gate_df * (y_df @ w_branch_diff[l])
        mix_out = merged @ w_out[l]
        x = x + gt_m[:, None, :] * rms_norm(mix_out, g_post_mix[l])

        h2 = modulate(rms_norm(x, g_pre_ffn[l]), sh_f, sc_f)
        ffn_out = moe_ffn(h2, w_router[l], b_router[l], w_gate_up[l], b_gate_up[l],
                          w_down[l], b_down[l])
        x = x + gt_f[:, None, :] * rms_norm(ffn_out, g_post_ffn[l])
    return x
```

```python
import math
import os
from contextlib import ExitStack
import numpy as np
import concourse.bass as bass
import concourse.mybir as mybir
from concourse.bass_utils import run_bass_kernel_spmd

F32 = mybir.dt.float32
BF16 = mybir.dt.bfloat16
AF = mybir.ActivationFunctionType
ALU = mybir.AluOpType
AX = mybir.AxisListType

D = 1024
KC = 8
EPS = 1e-6
LAM_INIT = 0.8 - 0.6 * math.exp(-0.3 * 0)
N_DMA_SEMS = 12


class Sched:
    CE = ("pe", "act", "dve", "pool")

    def __init__(self, nc, es):
        self.nc = nc
        self.q = {e: [] for e in self.CE + ("sp",)}
        self.cnt = {e: 0 for e in self.CE}
        self.sem = {e: es.enter_context(nc.semaphore("s_" + e)) for e in self.CE}
        self.dsem = [es.enter_context(nc.semaphore(f"d{i}")) for i in range(2 * N_DMA_SEMS)]
        self.dcnt = [0] * (2 * N_DMA_SEMS)
        self.dnext = {"sp": 0, "pool": 0}
        self.waited = {e: {} for e in self.q}
        self.lastw = {}
        self.readers = {}
        self.out_tokens = []
        self.es = es
        self.swsem = {}
        self.swgen = {}

    def _need(self, eng, tok):
        kind, a, b = tok
        if kind == "swd":
            gen, total = b
            if self.waited[eng].get(("swd", a)) == gen:
                return
            self.waited[eng][("swd", a)] = gen
            self.q[eng].append(("waitsw", a, total))
            return
        if kind == "eng":
            if a == eng:
                if eng == "pe":
                    return
                if self.cnt[eng] - b > 3:
                    return
            key, val = ("eng", a), b + 1
        else:
            key, val = ("dma", a), b
        if self.waited[eng].get(key, 0) >= val:
            return
        self.waited[eng][key] = val
        self.q[eng].append(("wait", key, val))

    def _deps(self, eng, r, w):
        for res in r:
            t = self.lastw.get(res)
            if t is not None:
                self._need(eng, t)
        for res in w:
            t = self.lastw.get(res)
            if t is not None:
                self._need(eng, t)
            for t in self.readers.get(res, ()):
                self._need(eng, t)

    def _commit(self, tok, r, w):
        for res in r:
            self.readers.setdefault(res, []).append(tok)
        for res in w:
            self.lastw[res] = tok
            self.readers[res] = []

    def op(self, eng, fn, r=(), w=()):
        self._deps(eng, r, w)
        tok = ("eng", eng, self.cnt[eng])
        self.cnt[eng] += 1
        self.q[eng].append(("op", fn))
        self._commit(tok, r, w)
        return tok

    def dma(self, qeng, out, in_, r=(), w=(), is_out=False):
        i = self.dnext[qeng] + (N_DMA_SEMS if qeng == "pool" else 0)
        self.dnext[qeng] = (self.dnext[qeng] + 1) % N_DMA_SEMS
        if self.dcnt[i] > 0:
            self._need(qeng, ("dma", i, self.dcnt[i] * 16))
        self._deps(qeng, r, w)
        self.dcnt[i] += 1
        tok = ("dma", i, self.dcnt[i] * 16)
        self.q[qeng].append(("dma", out, in_, i))
        self._commit(tok, r, w)
        if is_out:
            self.out_tokens.append(tok)
        return tok

    def dma_sw(self, key, pairs, r=(), w=()):
        for o, i in pairs:
            tok = self.dma("pool", o, i, r=r, w=w)
        return tok

    def alias(self, news, olds):
        toks = []
        for o in olds:
            if o in self.lastw:
                toks.append(self.lastw[o])
            toks += self.readers.get(o, [])
        for n in news:
            self.readers[n] = self.readers.get(n, []) + toks

    def finish(self, eng="sp"):
        for t in self.out_tokens:
            self._need(eng, t)

    def replay(self, eng, e):
        for item in self.q[eng]:
            if item[0] == "wait":
                key, val = item[1], item[2]
                sem = self.sem[key[1]] if key[0] == "eng" else self.dsem[key[1]]
                e.wait_ge(sem, val)
            elif item[0] == "waitsw":
                e.wait_ge(self.swsem[item[1]], item[2])
            elif item[0] == "clear":
                e.sem_clear(self.swsem[item[1]])
            elif item[0] == "swdma":
                e.dma_start(out=item[1], in_=item[2]).then_inc(self.swsem[item[3]], 16)
            elif item[0] == "op":
                item[1](e).then_inc(self.sem[eng], 1)
            else:
                e.dma_start(out=item[1], in_=item[2]).then_inc(self.dsem[item[3]], 16)


def build(S, E, TG=None, dbg=False, upto=99):
    if TG is None:
        TG = min(2048, S)
    NB = S // 128
    NT = S // 512
    NG = S // TG
    GB = TG // 128
    GT = TG // 512
    assert S % TG == 0 and TG % 512 == 0
    nc = bass.Bass("TRN2", target_bir_lowering=False)

    def din(name, shape, dt=F32):
        return nc.dram_tensor(name, list(shape), dt, kind="ExternalInput").ap()

    x_d = din("x", [S, D])
    cols_d = din("cols", [128, 56])
    bgu_d = din("bgu", [128, E * 16])
    rows_d = din("rows", [128, 4096 + 128 + 256 + E])
    wmod_d = din("w_mod", [D, 6 * D])
    win_d = din("w_in", [D, 5120])
    wchl_d = din("wch_l", [8, 128, 12 * 256])
    wout_d = din("w_out", [D, D])
    wr_d = din("w_router", [D, E])
    wgu_d = din("w_gu", [E, 2, 128, 8 * 1024])
    wdn_d = din("w_dn", [E, 2, 128, 4 * 1024])
    bdn_d = din("b_dn", [E, D])
    cst_d = din("cst", [128, 5 * 128 + 4])
    out_d = nc.dram_tensor("out", [S, D], F32, kind="ExternalOutput").ap()
    x1_d = nc.dram_tensor("x1s", [S, D], F32, kind="Internal").ap()
    dbg_d = {}
    if dbg:
        dbg_d["hT"] = nc.dram_tensor("dbg_hT", [128, KC * S], BF16, kind="ExternalOutput").ap()
        dbg_d["ysb"] = nc.dram_tensor("dbg_ysb", [128, 4 * S], BF16, kind="ExternalOutput").ap()
        dbg_d["ydf"] = nc.dram_tensor("dbg_ydf", [128, 4 * S], BF16, kind="ExternalOutput").ap()
        dbg_d["x1"] = nc.dram_tensor("dbg_x1", [S, D], F32, kind="ExternalOutput").ap()
        dbg_d["cw"] = nc.dram_tensor("dbg_cw", [128, NB * E], F32, kind="ExternalOutput").ap()

    win_v = win_d.rearrange("(kc p) c -> p kc c", p=128)

    es = ExitStack()
    with es:
        sc = Sched(nc, es)

        def finalize():
            sc.finish("sp")
            nc._sched_counts = {k: len(v) for k, v in sc.q.items()}
            with nc.Block() as block:
                @block.tensor
                def _(e):
                    sc.replay("pe", e)

                @block.scalar
                def _(e):
                    sc.replay("act", e)

                @block.vector
                def _(e):
                    sc.replay("dve", e)

                @block.gpsimd
                def _(e):
                    sc.replay("pool", e)

                @block.sync
                def _(e):
                    sc.replay("sp", e)

        def sb(name, shape, dt):
            return es.enter_context(nc.sbuf_tensor("t_" + name, list(shape), dt))

        hT = sb("hT", [128, KC, S], BF16)
        RB = max(8 * S, 2 * GB * D, 32768)
        regB = sb("regB", [128, RB], BF16)
        ysbT = regB[:, 0:4 * S].rearrange("p (c s) -> p c s", c=4)
        ydfT = regB[:, 4 * S:8 * S].rearrange("p (c s) -> p c s", c=4)
        CW = 29 * 1024
        regC = sb("regC", [128, CW], BF16)
        cst = sb("cst", [128, 5 * 128 + 4], F32)
        ident = cst[:, 0:128]
        mask_s = cst[:, 384:512]
        mask_i = cst[:, 512:640]
        wkey = cst[:, 640:644]
        cstb = sb("cstb", [128, 3 * 128], BF16)
        identb = cstb[:, 0:128]
        negU = cstb[:, 128:256]
        negL = cstb[:, 256:384]
        colt = sb("colt", [128, 56], F32)
        modc = sb("modc", [128, 48], F32)
        ggm = sb("ggm", [128, D], F32)
        ggf = sb("ggf", [128, D], F32)
        gsub = sb("gsub", [128, 128], F32)
        small = sb("small", [128, 64], F32)
        brt = sb("brt", [128, E], F32)
        cwt2 = sb("cwt2", [128, NB, E], F32)
        bgu = sb("bgut", [128, E * 16], F32)
        wrt = sb("wrt", [128, KC, E], F32)
        psw = [es.enter_context(nc.psum_tensor(f"psw{i}", [128, 1024], F32)) for i in range(4)]
        ps = [psw[i // 2][:, (i % 2) * 512:(i % 2 + 1) * 512] for i in range(8)]
        PS = [f"ps{i}" for i in range(8)]

        def carve(off_b, shape, dt):
            n = int(np.prod(shape[1:]))
            if dt == F32:
                v = regC[:, off_b // 2: off_b // 2 + 2 * n].bitcast(F32)
            else:
                v = regC[:, off_b // 2: off_b // 2 + n]
            if len(shape) == 3:
                v = v.rearrange("p (a b) -> p a b", a=shape[1])
            return v

        KB = 1024
        lam_neg = small[:, 0:1]

        sc.dma("sp", cst[:], cst_d[:, :], w=["cst"])
        sc.dma("sp", colt[:], cols_d[:, :], w=["colt"])
        sc.dma("sp", bgu[:], bgu_d[:, :], w=["bgu"])
        sc.dma("sp", wrt[:], wr_d.rearrange("(kc p) e -> p kc e", p=128), w=["wrt"])
        rowst = carve(0, [128, 4096 + 128 + 256 + E], F32)
        sc.dma("sp", rowst, rows_d[:, :], w=["rowst"])
        sc.op("dve", lambda e: e.tensor_copy(out=cstb[:], in_=cst[:, 0:384]), r=["cst"], w=["cstb"])
        ones = carve(18 * KB, [128, 128], F32)
        sc.op("dve", lambda e: e.memset(ones, 1.0), w=["ones"])
        sc.op("act", lambda e: e.activation(out=modc[:, 32:40], in_=colt[:, 0:8], func=AF.Silu), r=["colt"], w=["cact"])
        cbc = carve(19 * KB, [128, KC, 128], F32)
        for kc in range(KC):
            sc.op("dve", lambda e, kc=kc: e.tensor_scalar(out=cbc[:, kc, :], in0=ones, scalar1=modc[:, 32 + kc:33 + kc],
                                                         scalar2=None, op0=ALU.mult), r=["cact", "ones"], w=["cbc"])
        wm = [regB[:, i * 16384:(i + 1) * 16384].bitcast(F32).rearrange("p (k c) -> p k c", k=KC) for i in range(2)]
        wmod_v = wmod_d.rearrange("(kc p) c -> p kc c", p=128)
        col_secs = {0: 0, 1: 8, 3: 16, 4: 24}
        for sec in range(6):
            b = sec % 2
            for kc in range(KC):
                sc.dma("sp", wm[b][:, kc, :], wmod_v[:, kc, sec * D:(sec + 1) * D], w=[f"wm{b}_{kc}"])
            if sec in col_secs:
                for j in range(8):
                    for kc in range(KC):
                        sc.op("pe", lambda e, b=b, j=j, kc=kc, sec=sec: e.matmul(
                            ps[7][:, col_secs[sec] + j:col_secs[sec] + j + 1], lhsT=wm[b][:, kc, j * 128:(j + 1) * 128],
                            rhs=modc[:, 32 + kc:33 + kc], start=(kc == 0), stop=(kc == KC - 1), skip_group_check=True),
                            r=[f"wm{b}_{kc}", "cact"], w=[PS[7]])
            else:
                tgt, ro = (ggm, 0) if sec == 2 else (ggf, 1024)
                for hf in range(2):
                    pb = 5 + hf
                    for kc in range(KC):
                        sc.op("pe", lambda e, b=b, kc=kc, hf=hf, pb=pb, sec=sec: e.matmul(
                            ps[pb][:, :], lhsT=cbc[:, kc, :], rhs=wm[b][:, kc, hf * 512:(hf + 1) * 512],
                            start=(kc == 0), stop=(kc == KC - 1)), r=[f"wm{b}_{kc}", "cbc"], w=[PS[pb]])
                    sc.op("dve", lambda e, pb=pb, hf=hf, tgt=tgt, ro=ro: e.tensor_tensor(
                        out=tgt[:, hf * 512:(hf + 1) * 512], in0=ps[pb][:, :], in1=rowst[:, ro + hf * 512:ro + (hf + 1) * 512],
                        op=ALU.add), r=["rowst"], w=[PS[pb], "gg"])
                    sc.op("dve", lambda e, hf=hf, tgt=tgt, ro=ro: e.tensor_tensor(
                        out=tgt[:, hf * 512:(hf + 1) * 512], in0=tgt[:, hf * 512:(hf + 1) * 512],
                        in1=rowst[:, 2048 + ro + hf * 512:2048 + ro + (hf + 1) * 512], op=ALU.mult), r=["rowst"], w=["gg"])
        sc.op("dve", lambda e: e.tensor_tensor(out=modc[:, 0:32], in0=ps[7][:, 0:32], in1=colt[:, 8:40], op=ALU.add),
              r=["colt"], w=[PS[7], "modc"])
        sc.op("dve", lambda e: e.scalar_tensor_tensor(out=modc[:, 8:16], in0=modc[:, 8:16], scalar=1.0, in1=colt[:, 40:48],
                                                      op0=ALU.add, op1=ALU.mult), r=["colt"], w=["modc"])
        sc.op("dve", lambda e: e.scalar_tensor_tensor(out=modc[:, 24:32], in0=modc[:, 24:32], scalar=1.0, in1=colt[:, 48:56],
                                                      op0=ALU.add, op1=ALU.mult), r=["colt"], w=["modc"])
        sc.op("dve", lambda e: e.tensor_scalar(out=gsub[:], in0=rowst[:, 4096:4224], scalar1=float(1.0 - LAM_INIT), scalar2=None,
                                               op0=ALU.mult), r=["rowst"], w=["gsub"])
        sc.op("dve", lambda e: e.tensor_copy(out=brt[:], in_=rowst[:, 4480:4480 + E]), r=["rowst"], w=["brt"])
        lt = carve(23 * KB, [128, 128], F32)
        sc.op("dve", lambda e: e.tensor_tensor(out=lt[:, 0:64], in0=rowst[:, 4224:4288], in1=rowst[:, 4288:4352], op=ALU.mult),
              r=["rowst"], w=["lt"])
        sc.op("dve", lambda e: e.tensor_tensor(out=lt[:, 64:128], in0=rowst[:, 4352:4416], in1=rowst[:, 4416:4480], op=ALU.mult),
              r=["rowst"], w=["lt"])
        sc.op("dve", lambda e: e.reduce_sum(out=small[:, 1:2], in_=lt[:, 0:64], axis=AX.X), r=["lt"], w=["sm1"])
        sc.op("dve", lambda e: e.reduce_sum(out=small[:, 2:3], in_=lt[:, 64:128], axis=AX.X), r=["lt"], w=["sm2"])
        sc.op("act", lambda e: e.activation(out=small[:, 3:5], in_=small[:, 1:3], func=AF.Exp), r=["sm1", "sm2"], w=["sm3"])
        sc.op("dve", lambda e: e.scalar_tensor_tensor(out=lam_neg, in0=small[:, 4:5], scalar=float(-LAM_INIT), in1=small[:, 3:4],
                                                      op0=ALU.add, op1=ALU.subtract), r=["sm3"], w=["lam"])
        bgu3 = bgu[:].rearrange("p (e c) -> p e c", c=16)
        sc.op("dve", lambda e: e.tensor_scalar(out=bgu3[:, :, 8:16], in0=bgu3[:, :, 8:16], scalar1=1.0, scalar2=None, op0=ALU.add),
              r=["bgu"], w=["bgu"])

        if upto == 0:
            finalize()
            return nc
        xt = [carve((24 + 4 * i) * KB, [128, D], F32) for i in range(2)]
        junk = carve(32 * KB, [128, D], BF16)

        def rms_rstd(src_ap, n, dst, src_res, tag, jk, jres="junk"):
            sc.op("act", lambda e: e.activation(out=jk[:, 0:n], in_=src_ap, func=AF.Square, accum_out=dst),
                  r=src_res, w=[jres, tag])
            sc.op("dve", lambda e: e.tensor_scalar(out=dst, in0=dst, scalar1=1.0 / n, scalar2=EPS, op0=ALU.mult, op1=ALU.add),
                  w=[tag])
            sc.op("act", lambda e: e.activation(out=dst, in_=dst, func=AF.Ln), w=[tag])
            sc.op("act", lambda e: e.activation(out=dst, in_=dst, func=AF.Exp, scale=-0.5), w=[tag])

        def norm_transpose(xtile, xres, tb, sh0, gs0, want_f32=None, pbase=0):
            for g4 in range(2):
                pb = pbase + g4
                for j in range(4):
                    kc = g4 * 4 + j
                    sc.op("pe", lambda e, pb=pb, j=j, kc=kc: e.transpose(ps[pb][:, j * 128:(j + 1) * 128], xtile[:, kc * 128:(kc + 1) * 128],
                                                                      ident), r=[xres, "cst"], w=[PS[pb]])
                for j in range(4):
                    kc = g4 * 4 + j
                    sc.op("act", lambda e, pb=pb, j=j, kc=kc: e.activation(
                        out=hT[:, kc, tb * 128:(tb + 1) * 128], in_=ps[pb][:, j * 128:(j + 1) * 128], func=AF.Identity,
                        scale=modc[:, gs0 + kc:gs0 + kc + 1], bias=modc[:, sh0 + kc:sh0 + kc + 1]),
                        r=["modc"], w=[PS[pb], f"hT{tb // 4}"])
                    if want_f32 is not None:
                        sc.op("dve", lambda e, pb=pb, j=j, kc=kc: e.tensor_scalar(
                            out=want_f32[:, kc, :], in0=ps[pb][:, j * 128:(j + 1) * 128], scalar1=modc[:, gs0 + kc:gs0 + kc + 1],
                            scalar2=modc[:, sh0 + kc:sh0 + kc + 1], op0=ALU.mult, op1=ALU.add), r=["modc"], w=[PS[pb], "h2f"])

        for tb in range(NB + 1):
            if tb < NB:
                b = tb % 2
                sc.dma("sp", xt[b], x_d[tb * 128:(tb + 1) * 128, :], w=[f"xt{b}"])
                rms_rstd(xt[b], D, small[:, 8 + b:9 + b], [f"xt{b}"], f"rs{b}", junk)
                sc.op("dve", lambda e, b=b: e.tensor_scalar(out=xt[b], in0=xt[b], scalar1=small[:, 8 + b:9 + b], scalar2=None,
                                                             op0=ALU.mult), r=[f"rs{b}"], w=[f"xt{b}"])
            if tb >= 1:
                pb_ = (tb - 1) % 2
                norm_transpose(xt[pb_], f"xt{pb_}", tb - 1, 0, 8, pbase=2 * pb_)
        if dbg:
            sc.dma("sp", dbg_d["hT"], hT[:].rearrange("p k s -> p (k s)"), r=[f"hT{i}" for i in range(NT)], is_out=True)

        if upto == 1:
            finalize()
            return nc
        qT = carve(0, [128, S], BF16)
        kT = carve(2 * S, [128, S], BF16)
        vt = carve(4 * S, [128, NB, 130], BF16)
        o_w = 4 * S + 260 * NB
        o_w = (o_w + 63) // 64 * 64
        wst = [carve(o_w + 6 * KB * i, [128, KC, 384], BF16) for i in range(2)]
        o_t = o_w + 12 * KB
        e_w = [carve(o_t + p * 4 * KB, [128, 2, 512], F32) for p in range(2)]
        o_t += 8 * KB
        sp_w = [carve(o_t + p * 2 * KB, [128, 2, 512], BF16) for p in range(2)]
        o_t += 4 * KB
        a_w = [carve(o_t + p * 2 * KB, [128, 2, 512], BF16) for p in range(2)]
        o_t += 4 * KB
        g_w = carve(o_t, [128, 2, 512], F32)
        o_t += 4 * KB
        assert o_t <= 2 * CW, o_t
        o_d = o_w + 12 * KB
        p_t = [carve(o_d + i * 512, [128, 256], BF16) for i in range(4)]
        o1_tt = [carve(o_d + 2 * KB + 512 * i, [128, 128], F32) for i in range(2)]
        o2_t = carve(o_d + 3 * KB, [128, 128], F32)
        yb_t = [carve(o_d + 3 * KB + 512 + 256 * i, [128, 128], BF16) for i in range(2)]
        onesb = carve(o_d + 4 * KB, [128, NB], F32)

        WM = [f"wm{i}_{k}" for i in range(2) for k in range(KC)]
        P01 = ["rowst", "ones", "cbc", "lt", "xt0", "xt1", "junk"]
        SBT = ["e0", "e1", "sp0", "sp1", "a0", "a1", "g"]
        sc.alias(["qT", "kT", "vt", "wst0", "wst1"] + SBT, P01)
        sc.alias(["ysbT", "ydfT"], WM)
        wst_n = [0]

        def load_w3(c_q, c_k, c_v):
            b = wst_n[0] % 2
            wst_n[0] += 1
            sc.dma_sw(f"wst{b}", [(wst[b][:, :, i * 128:(i + 1) * 128], win_v[:, :, c0:c0 + 128]) for i, c0 in enumerate((c_q, c_k, c_v))],
                      w=[f"wst{b}"])
            return b

        def project(b, v_scale_col):
            for T in range(NT):
                tok = slice(T * 512, (T + 1) * 512)
                for which, dst, pb in ((0, qT, 0), (1, kT, 1)):
                    for kc in range(KC):
                        sc.op("pe", lambda e, kc=kc, which=which, pb=pb, tok=tok: e.matmul(
                            ps[pb][:, :], lhsT=wst[b][:, kc, which * 128:(which + 1) * 128], rhs=hT[:, kc, tok],
                            start=(kc == 0), stop=(kc == KC - 1)), r=[f"wst{b}", f"hT{T}"], w=[PS[pb]])
                    if which == 0:
                        sc.op("act", lambda e, pb=pb, tok=tok: e.activation(out=qT[:, tok], in_=ps[pb][:, :], func=AF.Copy, scale=0.125),
                              w=[PS[pb], "qT"])
                    else:
                        sc.op("dve", lambda e, pb=pb, tok=tok: e.tensor_copy(out=kT[:, tok], in_=ps[pb][:, :]), w=[PS[pb], "kT"])
                pb = 2 + (T % 2)
                for j in range(4):
                    blk = T * 4 + j
                    for kc in range(KC):
                        sc.op("pe", lambda e, kc=kc, j=j, blk=blk, pb=pb: e.matmul(
                            ps[pb][:, j * 128:(j + 1) * 128], lhsT=hT[:, kc, blk * 128:(blk + 1) * 128], rhs=wst[b][:, kc, 256:384],
                            start=(kc == 0), stop=(kc == KC - 1), skip_group_check=True), r=[f"wst{b}", f"hT{T}"], w=[PS[pb]])
                for j in range(4):
                    blk = T * 4 + j
                    if v_scale_col is None:
                        sc.op("dve", lambda e, j=j, blk=blk, pb=pb: e.tensor_copy(out=vt[:, blk, 0:128], in_=ps[pb][:, j * 128:(j + 1) * 128]),
                              w=[PS[pb], "vt"])
                    else:
                        sc.op("dve", lambda e, j=j, blk=blk, pb=pb: e.tensor_scalar(
                            out=vt[:, blk, 0:128], in0=ps[pb][:, j * 128:(j + 1) * 128], scalar1=v_scale_col, scalar2=None, op0=ALU.mult),
                            r=["cst"], w=[PS[pb], "vt"])

        nb = load_w3(0, 512, 1024)
        for hp in range(4):
            b = nb
            project(b, None)
            if hp < 3:
                nb = load_w3((hp + 1) * 128, 512 + (hp + 1) * 128, 1024 + (hp + 1) * 128)
            else:
                nb = load_w3(1536, 2048, 2560)
            steps = []
            for QT in range(NT):
                nsteps = QT * 4 + 4
                for st in range(nsteps):
                    steps.append((QT, st, nsteps))

            def sb_ctx(i, hd):
                QT, st, nsteps = steps[i]
                kb = nsteps - 1 - st
                c0 = max(0, kb - QT * 4) * 128
                par = i % 2
                return dict(QT=QT, st=st, nsteps=nsteps, kb=kb, c0=c0, cs=slice(c0, 512), par=par, q0=QT * 512,
                            rows=slice(hd * 64, (hd + 1) * 64), zb=par * 2 + hd, rb=4 + hd, ob=6 + (QT % 2), hd=hd)

            def st_A(c):
                sc.op("pe", lambda e, c=c: e.matmul(ps[c["zb"]][:, c["cs"]], lhsT=kT[c["rows"], c["kb"] * 128:(c["kb"] + 1) * 128],
                                                    rhs=qT[c["rows"], c["q0"] + c["c0"]:c["q0"] + 512], start=True, stop=True),
                      r=["qT", "kT"], w=[PS[c["zb"]]])

            def st_B(c):
                par, cs, c0 = c["par"], c["cs"], c["c0"]
                zw = psw[par].rearrange("p (h c) -> p h c", h=2)
                sc.op("act", lambda e: e.activation(out=e_w[par][:, :, cs], in_=zw[:, :, cs], func=AF.Exp),
                      w=[PS[par * 2], PS[par * 2 + 1], f"e{par}"])
                if c["kb"] >= c["QT"] * 4:
                    for hd in range(2):
                        sc.op("pool", lambda e, hd=hd: e.tensor_tensor(out=e_w[par][:, hd, c0:c0 + 128], in0=e_w[par][:, hd, c0:c0 + 128],
                                                                    in1=mask_s, op=ALU.mult), r=["cst"], w=[f"e{par}"])
                sc.op("act", lambda e: e.activation(out=sp_w[par][:, :, cs], in_=e_w[par][:, :, cs], func=AF.Ln, bias=1.0),
                      r=[f"e{par}"], w=[f"sp{par}"])

            def st_C1(c):
                par, cs = c["par"], c["cs"]
                sc.op("pe", lambda e, c=c: e.matmul(ps[c["rb"]][:, cs], lhsT=negU, rhs=sp_w[par][:, c["hd"], cs], start=(c["st"] == 0), stop=False,
                                                    skip_group_check=True), r=[f"sp{par}", "cstb"], w=[PS[c["rb"]]])

            def st_C2(c):
                cs = c["cs"]
                rw = psw[2].rearrange("p (h c) -> p h c", h=2)
                sc.op("act", lambda e: e.activation(out=g_w[:, :, cs], in_=rw[:, :, cs], func=AF.Exp), w=[PS[4], PS[5], "g"])

            def st_C3(c):
                par, cs = c["par"], c["cs"]
                if c["kb"] > 0:
                    sc.op("pe", lambda e, c=c: e.matmul(ps[c["rb"]][:, cs], lhsT=negL, rhs=sp_w[par][:, c["hd"], cs], start=False, stop=False,
                                                        skip_group_check=True), r=[f"sp{par}", "cstb"], w=[PS[c["rb"]]])

            def st_D(c):
                par, cs = c["par"], c["cs"]
                sc.op("dve", lambda e: e.tensor_tensor(out=a_w[par][:, :, cs], in0=e_w[par][:, :, cs], in1=g_w[:, :, cs], op=ALU.mult),
                      r=[f"e{par}", "g"], w=[f"a{par}"])

            def st_E(c):
                par, cs = c["par"], c["cs"]
                sc.op("pe", lambda e, c=c: e.matmul(ps[c["ob"]][c["rows"], cs], lhsT=vt[:, c["kb"], c["rows"]], rhs=a_w[par][:, c["hd"], cs],
                                                    start=(c["st"] == 0), stop=(c["st"] == c["nsteps"] - 1), skip_group_check=True),
                      r=[f"a{par}", "vt"], w=[PS[c["ob"]]])

            n = len(steps)
            for it in range(n + 3):
                if 0 <= it - 2 < n:
                    for hd in range(2):
                        st_C1(sb_ctx(it - 2, hd))
                    st_C2(sb_ctx(it - 2, 0))
                if it < n:
                    for hd in range(2):
                        st_A(sb_ctx(it, hd))
                if 0 <= it - 3 < n:
                    for hd in range(2):
                        st_E(sb_ctx(it - 3, hd))
                    c = sb_ctx(it - 3, 0)
                    if c["st"] == c["nsteps"] - 1:
                        sc.op("dve", lambda e, c=c, hp=hp: e.tensor_copy(out=ysbT[:, hp, c["q0"]:c["q0"] + 512], in_=ps[c["ob"]][:, :]),
                              w=[PS[c["ob"]], "ysbT"])
                if 0 <= it - 2 < n:
                    for hd in range(2):
                        st_C3(sb_ctx(it - 2, hd))
                    st_D(sb_ctx(it - 2, 0))
                if 0 <= it - 1 < n:
                    st_B(sb_ctx(it - 1, 0))
        if dbg:
            sc.dma("sp", dbg_d["ysb"], regB[:, 0:4 * S], r=["ysbT"], is_out=True)

        if upto == 2:
            finalize()
            return nc
        sc.alias(["p0", "p1", "p2", "p3", "o10", "o11", "o2", "yb0", "yb1", "onesb"], SBT)
        sc.op("dve", lambda e: e.memset(onesb, 1.0), w=["onesb"])
        for h in range(4):
            b = nb
            slope = 2.0 ** (-2.0 * (h + 1))
            project(b, wkey[:, h:h + 1])
            sc.op("dve", lambda e, h=h: e.tensor_scalar(out=vt[:, :, 128], in0=onesb, scalar1=wkey[:, h:h + 1], scalar2=None, op0=ALU.mult),
                  r=["onesb", "cst"], w=["vt"])
            if h < 3:
                nb = load_w3(1536 + (h + 1) * 128, 2048 + (h + 1) * 128, 2560 + (h + 1) * 128)
            dsteps = [(qb, st) for qb in range(NB) for st in range(qb + 1)]

            def df_ctx(i):
                qb, st = dsteps[i]
                kb = qb - st
                return dict(qb=qb, st=st, kb=kb, zb=i % 2, pt=p_t[i % 4], ptag=f"p{i % 4}", ob=4 + (qb % 2),
                            qs=slice(qb * 128, (qb + 1) * 128), bias=float(-slope * 128.0 * (qb - kb)))

            def df_A(c):
                for m in range(2):
                    rows = slice(m * 64, (m + 1) * 64)
                    sc.op("pe", lambda e, c=c, m=m, rows=rows: e.matmul(ps[2 * c["zb"] + m][:, 0:128], lhsT=kT[rows, c["kb"] * 128:(c["kb"] + 1) * 128],
                                                                     rhs=qT[rows, c["qs"]], start=True, stop=True),
                          r=["qT", "kT"], w=[PS[2 * c["zb"] + m]])

            def df_B(c):
                zw = psw[c["zb"]].rearrange("p (m c) -> p m c", m=2)
                ptw = c["pt"].rearrange("p (m c) -> p m c", m=2)
                sc.op("act", lambda e, c=c: e.activation(out=ptw, in_=zw[:, :, 0:128], func=AF.Exp, bias=c["bias"]),
                      w=[PS[2 * c["zb"]], PS[2 * c["zb"] + 1], c["ptag"]])
                if c["kb"] == c["qb"]:
                    for m in range(2):
                        sc.op("pool", lambda e, c=c, m=m: e.tensor_tensor(out=c["pt"][:, m * 128:(m + 1) * 128], in0=c["pt"][:, m * 128:(m + 1) * 128],
                                                                       in1=mask_i, op=ALU.mult), r=["cst"], w=[c["ptag"]])

            def df_C(c):
                for m in range(2):
                    sc.op("pe", lambda e, c=c, m=m: e.matmul(ps[c["ob"]][:, m * 256:m * 256 + 129], lhsT=c["pt"][:, m * 128:(m + 1) * 128],
                                                             rhs=vt[:, c["kb"], 0:129], start=(c["st"] == 0 and m == 0), stop=(c["st"] == c["qb"]),
                                                             skip_group_check=True), r=[c["ptag"], "vt"], w=[PS[c["ob"]]])

            def df_epi1(c):
                ob = c["ob"]
                o1_t = o1_tt[c["qb"] % 2]
                r3 = small[:, 18 + (c["qb"] % 2):19 + (c["qb"] % 2)]
                o1n = f"o1{c['qb'] % 2}"
                r3n = f"r3{c['qb'] % 2}"
                sc.op("dve", lambda e: e.reciprocal(out=small[:, 16:17], in_=ps[ob][:, 128:129]), w=[PS[ob], "r1"])
                sc.op("dve", lambda e: e.reciprocal(out=small[:, 17:18], in_=ps[ob][:, 384:385]), w=[PS[ob], "r2"])
                sc.op("dve", lambda e: e.tensor_tensor(out=small[:, 17:18], in0=small[:, 17:18], in1=lam_neg, op=ALU.mult), r=["lam"], w=["r2"])
                sc.op("dve", lambda e: e.tensor_scalar(out=o1_t, in0=ps[ob][:, 0:128], scalar1=small[:, 16:17], scalar2=None, op0=ALU.mult),
                      r=["r1"], w=[PS[ob], o1n])
                sc.op("dve", lambda e: e.scalar_tensor_tensor(out=o1_t, in0=ps[ob][:, 256:384], scalar=small[:, 17:18], in1=o1_t,
                                                              op0=ALU.mult, op1=ALU.add), r=["r2"], w=[PS[ob], o1n])
                sc.op("dve", lambda e: e.tensor_tensor(out=o2_t, in0=o1_t, in1=o1_t, op=ALU.mult), r=[o1n], w=["o2"])
                sc.op("dve", lambda e: e.reduce_sum(out=r3, in_=o2_t, axis=AX.X), r=["o2"], w=[r3n])
                sc.op("dve", lambda e: e.tensor_scalar(out=r3, in0=r3, scalar1=1.0 / 128, scalar2=EPS,
                                                       op0=ALU.mult, op1=ALU.add), w=[r3n])

            def df_epi2(c):
                qb = c["qb"]
                o1_t = o1_tt[qb % 2]
                r3 = small[:, 18 + (qb % 2):19 + (qb % 2)]
                o1n = f"o1{qb % 2}"
                r3n = f"r3{qb % 2}"
                sc.op("act", lambda e: e.activation(out=r3, in_=r3, func=AF.Ln), w=[r3n])
                sc.op("act", lambda e: e.activation(out=r3, in_=r3, func=AF.Exp, scale=-0.5), w=[r3n])
                yb = yb_t[qb % 2]
                sc.op("dve", lambda e: e.scalar_tensor_tensor(out=yb, in0=o1_t, scalar=r3, in1=gsub[:],
                                                              op0=ALU.mult, op1=ALU.mult), r=[o1n, r3n, "gsub"], w=[f"yb{qb % 2}"])
                tb_ = 6 + (qb % 2)
                psb = ps[tb_][:, 0:256].bitcast(BF16)
                sc.op("pe", lambda e: e.transpose(psb[:, 0:128], yb, identb), r=[f"yb{qb % 2}", "cstb"], w=[PS[tb_]])
                sc.op("dve", lambda e, h=h: e.tensor_copy(out=ydfT[:, h, c["qs"]], in_=psb[:, 0:128]), w=[PS[tb_], "ydfT"])

            n = len(dsteps)
            pend = []
            for it in range(n + 6):
                if it < n:
                    df_A(df_ctx(it))
                if 0 <= it - 1 < n:
                    df_B(df_ctx(it - 1))
                if 0 <= it - 2 < n:
                    c = df_ctx(it - 2)
                    df_C(c)
                    if c["st"] == c["qb"]:
                        while pend and pend[0][1]["qb"] <= c["qb"] - 2:
                            df_epi2(pend.pop(0)[1])
                        df_epi1(c)
                        pend.append((it + 3, c))
                while pend and pend[0][0] <= it:
                    df_epi2(pend.pop(0)[1])
            assert not pend
        if dbg:
            sc.dma("sp", dbg_d["ydf"], regB[:, 4 * S:8 * S], r=["ydfT"], is_out=True)

        if upto == 3:
            finalize()
            return nc
        wbr = carve(0, [128, 8, 1024], BF16)
        wch2d = [carve(16 * KB + 6 * KB * i, [128, 12 * 256], BF16) for i in range(2)]
        wch = [w2.rearrange("p (a b) -> p a b", a=12) for w2 in wch2d]
        mrg = carve(28 * KB, [128, 8, 512], BF16)
        s1 = carve(36 * KB, [128, 512], F32)
        s2 = carve(38 * KB, [128, 512], F32)
        m1 = carve(40 * KB, [128, 512], F32)
        xt3 = [carve((42 + 4 * i) * KB, [128, D], F32) for i in range(2)]
        junk3 = carve(50 * KB, [128, D], BF16)
        h2f = carve(52 * KB, [128, KC, 128], F32)
        P3 = ["qT", "kT", "vt", "wst0", "wst1", "e0", "e1", "sp0", "sp1", "a0", "a1", "g", "p0", "p1", "p2", "p3", "o10", "o11", "o2", "yb0", "yb1", "onesb"]
        sc.alias(["wbr", "wch0", "wch1", "mrg", "s1", "s2", "m1", "x30", "x31", "junk", "h2f"], P3)
        wout_v = wout_d.rearrange("(kc p) c -> p kc c", p=128)
        sc.dma_sw("wbr", [(wbr[:, kc, :], wout_v[:, kc, :]) for kc in range(KC)], w=["wbr"])
        wch_n = [0]

        def load_wch(fc):
            b = wch_n[0] % 2
            wch_n[0] += 1
            sc.dma_sw(f"wch{b}", [(wch2d[b], wchl_d[fc])], w=[f"wch{b}"])
            return b

        nbw = load_wch(0)
        for T in range(NT):
            tok = slice(T * 512, (T + 1) * 512)
            for fc in range(8):
                b = nbw
                nxt = (T * 8 + fc + 1)
                if nxt < NT * 8:
                    nbw = load_wch(nxt % 8)
                s4 = 4 * (fc % 2)
                for gi, pb in ((0, s4), (1, s4 + 1)):
                    for kc in range(KC):
                        sc.op("pe", lambda e, b=b, gi=gi, pb=pb, kc=kc, tok=tok: e.matmul(
                            ps[pb][:, :], lhsT=wch[b][:, kc, gi * 128:(gi + 1) * 128], rhs=hT[:, kc, tok], start=(kc == 0), stop=(kc == KC - 1)),
                            r=[f"wch{b}", f"hT{T}"], w=[PS[pb]])
                for bi, pb, ysrc, yres in ((0, s4 + 2, ysbT, "ysbT"), (1, s4 + 3, ydfT, "ydfT")):
                    for c in range(4):
                        sc.op("pe", lambda e, b=b, bi=bi, pb=pb, c=c, ysrc=ysrc, tok=tok: e.matmul(
                            ps[pb][:, :], lhsT=wch[b][:, 8 + c, bi * 128:(bi + 1) * 128], rhs=ysrc[:, c, tok], start=(c == 0), stop=(c == 3)),
                            r=[f"wch{b}", yres], w=[PS[pb]])
                sc.op("act", lambda e, s4=s4: e.activation(out=s1, in_=ps[s4][:, :], func=AF.Sigmoid), w=[PS[s4], "s1"])
                sc.op("act", lambda e, s4=s4: e.activation(out=s2, in_=ps[s4 + 1][:, :], func=AF.Sigmoid), w=[PS[s4 + 1], "s2"])
                sc.op("dve", lambda e, s4=s4: e.tensor_tensor(out=m1, in0=s1, in1=ps[s4 + 2][:, :], op=ALU.mult), r=["s1"], w=[PS[s4 + 2], "m1"])
                sc.op("dve", lambda e, s4=s4: e.tensor_tensor(out=s2, in0=s2, in1=ps[s4 + 3][:, :], op=ALU.mult), w=[PS[s4 + 3], "s2"])
                sc.op("pool", lambda e, fc=fc: e.tensor_tensor(out=mrg[:, fc, :], in0=m1, in1=s2, op=ALU.add), r=["m1", "s2"], w=["mrg"])
            def Q1(j):
                tb = T * 4 + j
                b2 = tb % 2
                ob0 = 4 * (j % 2)
                ss0 = 20 if j % 2 == 0 else 28
                sc.dma("sp", xt3[b2], x_d[tb * 128:(tb + 1) * 128, :], w=[f"x3{b2}"])
                for hf in range(2):
                    pb = ob0 + hf
                    for fc in range(8):
                        sc.op("pe", lambda e, pb=pb, fc=fc, hf=hf: e.matmul(
                            ps[pb][:, :], lhsT=mrg[:, fc, j * 128:(j + 1) * 128], rhs=wbr[:, fc, hf * 512:(hf + 1) * 512], start=(fc == 0),
                            stop=(fc == 7)), r=["mrg", "wbr"], w=[PS[pb]])
                    sc.op("act", lambda e, pb=pb, hf=hf: e.activation(out=junk3[:, 0:512], in_=ps[pb][:, :], func=AF.Square,
                                                                   accum_out=small[:, ss0 + hf:ss0 + hf + 1]), w=[PS[pb], "junk", f"ssm{j % 2}{hf}"])

            def Q2(j):
                tb = T * 4 + j
                b2 = tb % 2
                ob0 = 4 * (j % 2)
                ss0 = 20 if j % 2 == 0 else 28
                sc.op("dve", lambda e: e.tensor_tensor(out=small[:, 22:23], in0=small[:, ss0:ss0 + 1], in1=small[:, ss0 + 1:ss0 + 2], op=ALU.add),
                      r=[f"ssm{j % 2}0", f"ssm{j % 2}1"], w=["rsm"])
                sc.op("dve", lambda e: e.tensor_scalar(out=small[:, 22:23], in0=small[:, 22:23], scalar1=1.0 / D, scalar2=EPS,
                                                       op0=ALU.mult, op1=ALU.add), w=["rsm"])
                sc.op("act", lambda e: e.activation(out=small[:, 22:23], in_=small[:, 22:23], func=AF.Ln), w=["rsm"])
                sc.op("act", lambda e: e.activation(out=small[:, 22:23], in_=small[:, 22:23], func=AF.Exp, scale=-0.5), w=["rsm"])
                for hf in range(2):
                    pb = ob0 + hf
                    hs = slice(hf * 512, (hf + 1) * 512)
                    sc.op("dve", lambda e, pb=pb, hs=hs: e.scalar_tensor_tensor(out=m1, in0=ps[pb][:, :], scalar=small[:, 22:23], in1=ggm[:, hs],
                                                                             op0=ALU.mult, op1=ALU.mult), r=["rsm", "gg"], w=[PS[pb], "m1"])
                    sc.op("pool", lambda e, hs=hs, b2=b2: e.tensor_tensor(out=xt3[b2][:, hs], in0=xt3[b2][:, hs], in1=m1, op=ALU.add),
                          r=["m1"], w=[f"x3{b2}"])
                sc.dma("sp", x1_d[tb * 128:(tb + 1) * 128, :], xt3[b2], r=[f"x3{b2}"], w=["x1d"])
                if dbg:
                    sc.dma("sp", dbg_d["x1"][tb * 128:(tb + 1) * 128, :], xt3[b2], r=[f"x3{b2}"], is_out=True)
                rms_rstd(xt3[b2], D, small[:, 24:25], [f"x3{b2}"], "rs3", junk3)
                sc.op("dve", lambda e, b2=b2: e.tensor_scalar(out=xt3[b2], in0=xt3[b2], scalar1=small[:, 24:25], scalar2=None, op0=ALU.mult),
                      r=["rs3"], w=[f"x3{b2}"])
                norm_transpose(xt3[b2], f"x3{b2}", tb, 16, 24, want_f32=h2f, pbase=2)
                for kc in range(KC):
                    sc.op("pe", lambda e, kc=kc: e.matmul(ps[6][:, 0:E], lhsT=h2f[:, kc, :], rhs=wrt[:, kc, :], start=(kc == 0),
                                                          stop=(kc == KC - 1)), r=["h2f", "wrt"], w=[PS[6]])
                lg = small[:, 32:32 + E]
                sc.op("dve", lambda e: e.tensor_tensor(out=lg, in0=ps[6][:, 0:E], in1=brt[:], op=ALU.add), r=["brt"], w=[PS[6], "lg"])
                sc.op("dve", lambda e: e.max(out=modc[:, 40:48], in_=lg), r=["lg"], w=["mx8"])
                sc.op("dve", lambda e: e.tensor_scalar(out=small[:, 25:26], in0=modc[:, 40:41], scalar1=-1.0, scalar2=None, op0=ALU.mult),
                      r=["mx8"], w=["nmx"])
                ex = m1[:, 0:E]
                sc.op("act", lambda e: e.activation(out=ex, in_=lg, func=AF.Exp, bias=small[:, 25:26]), r=["lg", "nmx"], w=["m1"])
                sc.op("dve", lambda e, tb=tb: e.scalar_tensor_tensor(out=cwt2[:, tb, :], in0=lg, scalar=modc[:, 43:44], in1=ex, op0=ALU.is_ge,
                                                                   op1=ALU.mult), r=["lg", "mx8", "m1"], w=["cwt2"])
                sc.op("dve", lambda e, tb=tb: e.reduce_sum(out=small[:, 26:27], in_=cwt2[:, tb, :], axis=AX.X), r=["cwt2"], w=["den"])
                sc.op("dve", lambda e: e.reciprocal(out=small[:, 26:27], in_=small[:, 26:27]), w=["den"])
                sc.op("dve", lambda e, tb=tb: e.tensor_scalar(out=cwt2[:, tb, :], in0=cwt2[:, tb, :], scalar1=small[:, 26:27],
                                                              scalar2=float(1.0 / 1.702), op0=ALU.mult, op1=ALU.mult), r=["den"], w=["cwt2"])
            Q1(0)
            for j in range(4):
                if j < 3:
                    Q1(j + 1)
                Q2(j)
        if dbg:
            sc.dma("sp", dbg_d["cw"], cwt2[:].rearrange("p a b -> p (a b)"), r=["cwt2"], is_out=True)

        if upto == 4:
            finalize()
            return nc
        acc = regB[:, 0:2 * GB * D].bitcast(F32).rearrange("p (a b) -> p a b", a=GB)
        wgu2d = [carve(16 * KB * i, [128, KC * 1024], BF16) for i in range(2)]
        wgu = [w2.rearrange("p (a b) -> p a b", a=KC) for w2 in wgu2d]
        wdn2d = [carve(32 * KB, [128, 4 * 1024], BF16)]
        wdn = [w2.rearrange("p (a b) -> p a b", a=4) for w2 in wdn2d]
        actT = [carve(40 * KB + 4 * KB * i, [128, 4, 512], BF16) for i in range(2)]
        gtm = [carve(48 * KB + 2 * KB * i, [128, 512], F32) for i in range(2)]
        atm = [carve(52 * KB, [128, 512], F32)] * 2
        ctm = [carve(54 * KB + 2 * KB * i, [128, 512], F32) for i in range(2)]
        x1t = carve(48 * KB, [128, D], F32)
        junk4 = carve(52 * KB, [128, D], BF16)
        cwT = carve(52 * KB, [128, 128], F32)
        bdn = carve(54 * KB, [128, D], F32)
        assert 58 * KB <= 2 * CW
        P4 = ["wbr", "wch0", "wch1", "mrg", "s1", "s2", "m1", "x30", "x31", "h2f", "junk", "ysbT", "ydfT"] + WM
        sc.alias(["wgu0", "wgu1", "wdn0", "actT0", "actT1", "gt0", "gt1", "at0", "ct0", "ct1", "junk"] + [f"acc{j}" for j in range(GB)], P4)
        def load_wgu(e_, hf, b):
            sc.dma_sw(f"wgu{b}", [(wgu2d[b][:, q * 2048:(q + 1) * 2048], wgu_d[e_, hf, :, q * 2048:(q + 1) * 2048]) for q in range(4)],
                      w=[f"wgu{b}"])

        def load_wdn(e_, hf):
            sc.dma_sw("wdn0", [(wdn2d[0][:, q * 2048:(q + 1) * 2048], wdn_d[e_, hf, :, q * 2048:(q + 1) * 2048]) for q in range(2)],
                      w=["wdn0"])

        units = [(g, e_, hf) for g in range(NG) for e_ in range(E) for hf in range(2)]
        load_wgu(0, 0, 0)
        ucount = 0
        acount = 0
        for g in range(NG):
            sc.dma("sp", bdn[0:E, :], bdn_d[:, :], w=["ct0", "ct1"])
            sc.op("act", lambda e: e.activation(out=bdn[0:E, :], in_=bdn[0:E, :], func=AF.Copy, scale=1.702), w=["ct0", "ct1"])
            for jb in range(GB):
                tb = g * GB + jb
                sc.op("pe", lambda e, tb=tb: e.transpose(ps[6][0:E, 0:128], cwt2[:, tb, :], ident), r=["cwt2", "cst"], w=[PS[6]])
                sc.op("dve", lambda e: e.tensor_copy(out=cwT[0:E, :], in_=ps[6][0:E, 0:128]), w=[PS[6], "at0"])
                for hf in range(2):
                    sc.op("pe", lambda e, hf=hf: e.matmul(ps[7][:, :], lhsT=cwT[0:E, :], rhs=bdn[0:E, hf * 512:(hf + 1) * 512], start=True,
                                                          stop=True), r=["at0", "ct0", "ct1"], w=[PS[7]])
                    sc.op("act", lambda e, jb=jb, hf=hf: e.activation(out=acc[:, jb, hf * 512:(hf + 1) * 512], in_=ps[7][:, :], func=AF.Copy),
                          w=[PS[7], f"acc{jb}"])
            def moe_G(u):
                e_, hf, Tl, b, ab = u
                T = g * GT + Tl
                tok = slice(T * 512, (T + 1) * 512)
                for fcl in range(4):
                    pg = (fcl % 2) * 2
                    pu = pg + 1
                    for kc in range(KC):
                        sc.op("pe", lambda e, kc=kc, fcl=fcl, pg=pg: e.matmul(
                            ps[pg][:, :], lhsT=wgu[b][:, kc, fcl * 128:(fcl + 1) * 128], rhs=hT[:, kc, tok], start=(kc == 0),
                            stop=(kc == KC - 1)), r=[f"wgu{b}", f"hT{T}"], w=[PS[pg]])
                    for kc in range(KC):
                        sc.op("pe", lambda e, kc=kc, fcl=fcl, pu=pu: e.matmul(
                            ps[pu][:, :], lhsT=wgu[b][:, kc, 512 + fcl * 128:512 + (fcl + 1) * 128], rhs=hT[:, kc, tok], start=(kc == 0),
                            stop=(kc == KC - 1)), r=[f"wgu{b}", f"hT{T}"], w=[PS[pu]])
                    tp = fcl % 2
                    cg = e_ * 16 + hf * 4 + fcl
                    cu = e_ * 16 + 8 + hf * 4 + fcl
                    sc.op("dve", lambda e, pg=pg, tp=tp, cg=cg: e.tensor_scalar(out=gtm[tp], in0=ps[pg][:, :], scalar1=bgu[:, cg:cg + 1],
                                                                               scalar2=7.0, op0=ALU.add, op1=ALU.min),
                          r=["bgu"], w=[PS[pg], f"gt{tp}"])
                    sc.op("act", lambda e, tp=tp: e.activation(out=gtm[tp], in_=gtm[tp], func=AF.Silu, scale=1.702), w=[f"gt{tp}"])
                    sc.op("dve", lambda e, pu=pu, tp=tp, cu=cu: e.tensor_scalar(out=atm[tp], in0=ps[pu][:, :], scalar1=bgu[:, cu:cu + 1],
                                                                               scalar2=-6.0, op0=ALU.add, op1=ALU.max),
                          r=["bgu"], w=[PS[pu], "at0"])
                    sc.op("dve", lambda e, tp=tp, fcl=fcl: e.scalar_tensor_tensor(out=actT[ab][:, fcl, :], in0=atm[tp], scalar=8.0,
                                                                                in1=gtm[tp], op0=ALU.min, op1=ALU.mult),
                          r=["at0", f"gt{tp}"], w=[f"actT{ab}"])

            def moe_D(u):
                e_, hf, Tl, b, ab = u
                for sub in range(4):
                    jb = Tl * 4 + sub
                    tb = g * GB + jb
                    for h2 in range(2):
                        pd = 4 + h2
                        cp = (sub * 2 + h2) % 2
                        for fcl in range(4):
                            sc.op("pe", lambda e, fcl=fcl, sub=sub, h2=h2, pd=pd: e.matmul(
                                ps[pd][:, :], lhsT=actT[ab][:, fcl, sub * 128:(sub + 1) * 128], rhs=wdn[0][:, fcl, h2 * 512:(h2 + 1) * 512],
                                start=(fcl == 0), stop=(fcl == 3)), r=[f"actT{ab}", "wdn0"], w=[PS[pd]])
                        if h2 == 0:
                            sc.op("dve", lambda e, pd=pd, jb=jb, tb=tb: e.scalar_tensor_tensor(
                                out=acc[:, jb, 0:512], in0=ps[pd][:, :], scalar=cwt2[:, tb, e_:e_ + 1], in1=acc[:, jb, 0:512],
                                op0=ALU.mult, op1=ALU.add), r=["cwt2"], w=[PS[pd], f"acc{jb}"])
                        else:
                            cp = sub % 2
                            sc.op("act", lambda e, pd=pd, cp=cp, tb=tb: e.activation(out=ctm[cp], in_=ps[pd][:, :], func=AF.Copy,
                                                                                 scale=cwt2[:, tb, e_:e_ + 1]),
                                  r=["cwt2"], w=[PS[pd], f"ct{cp}"])
                            sc.op("pool", lambda e, cp=cp, jb=jb: e.tensor_tensor(out=acc[:, jb, 512:1024], in0=acc[:, jb, 512:1024], in1=ctm[cp],
                                                                               op=ALU.add), r=[f"ct{cp}"], w=[f"acc{jb}"])

            gunits = []
            for e_ in range(E):
                for hf in range(2):
                    b = ucount % 2
                    ucount += 1
                    nxt = units[ucount][1:] if ucount < len(units) else None
                    for Tl in range(GT):
                        gunits.append(((e_, hf, Tl, b, acount % 2), nxt if Tl == 0 else None))
                        acount += 1
            i = 0
            while i < len(gunits):
                grp = gunits[i:i + GT]
                i += GT
                (e_, hf, _, b, _), nxt = grp[0]
                load_wdn(e_, hf)
                if nxt is not None:
                    load_wgu(nxt[0], nxt[1], 1 - b)
                moe_G(grp[0][0])
                for k in range(1, GT):
                    moe_G(grp[k][0])
                    moe_D(grp[k - 1][0])
                moe_D(grp[GT - 1][0])
            for jb in range(GB):
                tb = g * GB + jb
                b2 = tb % 2
                sc.dma("sp", x1t, x1_d[tb * 128:(tb + 1) * 128, :], r=["x1d"], w=["gt0", "gt1"])
                rms_rstd(acc[:, jb, :], D, small[:, 27:28], [f"acc{jb}"], "rs4", junk4, jres="at0")
                for hf in range(2):
                    hs = slice(hf * 512, (hf + 1) * 512)
                    sc.op("dve", lambda e, jb=jb, hs=hs: e.scalar_tensor_tensor(out=acc[:, jb, hs], in0=acc[:, jb, hs], scalar=small[:, 27:28],
                                                                             in1=ggf[:, hs], op0=ALU.mult, op1=ALU.mult),
                          r=["rs4", "gg"], w=[f"acc{jb}"])
                    sc.op("pool", lambda e, jb=jb, hs=hs: e.tensor_tensor(out=x1t[:, hs], in0=x1t[:, hs], in1=acc[:, jb, hs], op=ALU.add),
                          r=[f"acc{jb}"], w=["gt0", "gt1"])
                sc.dma("sp", out_d[tb * 128:(tb + 1) * 128, :], x1t, r=["gt0", "gt1"], is_out=True)
        finalize()
    return nc


def host_consts():
    c = np.zeros((128, 5 * 128 + 4), np.float32)
    i = np.arange(128)
    c[:, 0:128] = np.eye(128, dtype=np.float32)
    c[:, 128:256] = -(i[:, None] >= i[None, :]).astype(np.float32)
    c[:, 256:384] = -(i[:, None] < i[None, :]).astype(np.float32)
    c[:, 384:512] = (i[:, None] < i[None, :]).astype(np.float32)
    c[:, 512:640] = (i[:, None] <= i[None, :]).astype(np.float32)
    for h in range(4):
        slope = 2.0 ** (-2.0 * (h + 1))
        c[:, 640 + h] = np.exp(-slope * (127 - i)).astype(np.float32)
    return c


def col_layout(v):
    return np.ascontiguousarray(np.asarray(v, np.float32).reshape(-1, 128).T)


def make_in_maps(inputs, S, E, n_cores):
    f = lambda a: np.ascontiguousarray(np.asarray(a, np.float32))
    b_mod = f(inputs["b_mod"])[0]
    secs = [b_mod[i * D:(i + 1) * D] for i in range(6)]
    cst = host_consts()
    rows1 = np.concatenate([secs[2], secs[5], f(inputs["g_post_mix"])[0], f(inputs["g_post_ffn"])[0], f(inputs["g_subln"])[0],
                            f(inputs["lambda_q1"])[0], f(inputs["lambda_k1"])[0], f(inputs["lambda_q2"])[0], f(inputs["lambda_k2"])[0],
                            f(inputs["b_router"])[0]])
    rows = np.ascontiguousarray(np.broadcast_to(rows1[None, :], (128, rows1.size)))
    bgu = np.concatenate([col_layout(f(inputs["b_gate_up"])[0, e]) for e in range(E)], axis=1)
    w_in0 = f(inputs["w_in"])[0]
    wg = w_in0[:, 3072:5120].reshape(8, 128, 2, 8, 128).transpose(3, 1, 0, 2, 4).reshape(8, 128, 8 * 256)
    wb = np.stack([f(inputs["w_branch_sb"])[0], f(inputs["w_branch_diff"])[0]], axis=0).reshape(2, 4, 128, 8, 128)
    wb = wb.transpose(3, 2, 1, 0, 4).reshape(8, 128, 4 * 256)
    wch_l = np.ascontiguousarray(np.concatenate([wg, wb], axis=2))
    wgu_l = np.ascontiguousarray(f(inputs["w_gate_up"])[0].reshape(E, 8, 128, 2, 2, 512).transpose(0, 4, 2, 1, 3, 5)).reshape(E, 2, 128, 8192)
    wdn_l = np.ascontiguousarray(f(inputs["w_down"])[0].reshape(E, 2, 4, 128, 1024).transpose(0, 1, 3, 2, 4)).reshape(E, 2, 128, 4096)
    shared = {
        "bgu": np.ascontiguousarray(bgu), "rows": rows, "w_mod": f(inputs["w_mod"])[0], "w_in": f(inputs["w_in"])[0],
        "wch_l": wch_l, "w_out": f(inputs["w_out"])[0],
        "w_router": f(inputs["w_router"])[0], "w_gu": wgu_l, "w_dn": wdn_l,
        "b_dn": f(inputs["b_down"])[0], "cst": cst,
    }
    x = f(inputs["x"])
    c = f(inputs["c"])
    maps = []
    for b in range(n_cores):
        cols = np.concatenate([col_layout(c[b]), col_layout(secs[0]), col_layout(secs[1]), col_layout(secs[3]), col_layout(secs[4]),
                               col_layout(f(inputs["g_pre_mix"])[0]), col_layout(f(inputs["g_pre_ffn"])[0])], axis=1)
        m = dict(shared)
        m["x"] = np.ascontiguousarray(x[b])
        m["cols"] = np.ascontiguousarray(cols)
        maps.append(m)
    return maps


_NC_CACHE = {}


def kernel(**inputs):
    x = np.asarray(inputs["x"])
    B, S, _ = x.shape
    E = np.asarray(inputs["w_gate_up"]).shape[1]
    key = (S, E)
    if key not in _NC_CACHE:
        _NC_CACHE[key] = build(S, E)
    nc = _NC_CACHE[key]
    maps = make_in_maps(inputs, S, E, B)
    res = run_bass_kernel_spmd(nc, maps, core_ids=list(range(B)))
    return np.stack([np.asarray(r["out"], np.float32) for r in res.results], axis=0)
```

```python
import math
import os
from contextlib import ExitStack
import numpy as np
import concourse.bass as bass
import concourse.mybir as mybir
from concourse.bass_utils import run_bass_kernel_spmd

F32 = mybir.dt.float32
BF16 = mybir.dt.bfloat16
AF = mybir.ActivationFunctionType
ALU = mybir.AluOpType
AX = mybir.AxisListType

D = 1024
KC = 8
EPS = 1e-6
LAM_INIT = 0.8 - 0.6 * math.exp(-0.3 * 0)
N_DMA_SEMS = 12


class Sched:
    CE = ("pe", "act", "dve", "pool")

    def __init__(self, nc, es):
        self.nc = nc
        self.q = {e: [] for e in self.CE + ("sp",)}
        self.cnt = {e: 0 for e in self.CE}
        self.sem = {e: es.enter_context(nc.semaphore("s_" + e)) for e in self.CE}
        self.dsem = [es.enter_context(nc.semaphore(f"d{i}")) for i in range(2 * N_DMA_SEMS)]
        self.dcnt = [0] * (2 * N_DMA_SEMS)
        self.dnext = {"sp": 0, "pool": 0}
        self.waited = {e: {} for e in self.q}
        self.lastw = {}
        self.readers = {}
        self.out_tokens = []
        self.es = es
        self.swsem = {}
        self.swgen = {}

    def _need(self, eng, tok):
        kind, a, b = tok
        if kind == "swd":
            gen, total = b
            if self.waited[eng].get(("swd", a)) == gen:
                return
            self.waited[eng][("swd", a)] = gen
            self.q[eng].append(("waitsw", a, total))
            return
        if kind == "eng":
            if a == eng:
                if eng == "pe":
                    return
                if self.cnt[eng] - b > 3:
                    return
            key, val = ("eng", a), b + 1
        else:
            key, val = ("dma", a), b
        if self.waited[eng].get(key, 0) >= val:
            return
        self.waited[eng][key] = val
        self.q[eng].append(("wait", key, val))

    def _deps(self, eng, r, w):
        for res in r:
            t = self.lastw.get(res)
            if t is not None:
                self._need(eng, t)
        for res in w:
            t = self.lastw.get(res)
            if t is not None:
                self._need(eng, t)
            for t in self.readers.get(res, ()):
                self._need(eng, t)

    def _commit(self, tok, r, w):
        for res in r:
            self.readers.setdefault(res, []).append(tok)
        for res in w:
            self.lastw[res] = tok
            self.readers[res] = []

    def op(self, eng, fn, r=(), w=()):
        self._deps(eng, r, w)
        tok = ("eng", eng, self.cnt[eng])
        self.cnt[eng] += 1
        self.q[eng].append(("op", fn))
        self._commit(tok, r, w)
        return tok

    def dma(self, qeng, out, in_, r=(), w=(), is_out=False):
        i = self.dnext[qeng] + (N_DMA_SEMS if qeng == "pool" else 0)
        self.dnext[qeng] = (self.dnext[qeng] + 1) % N_DMA_SEMS
        if self.dcnt[i] > 0:
            self._need(qeng, ("dma", i, self.dcnt[i] * 16))
        self._deps(qeng, r, w)
        self.dcnt[i] += 1
        tok = ("dma", i, self.dcnt[i] * 16)
        self.q[qeng].append(("dma", out, in_, i))
        self._commit(tok, r, w)
        if is_out:
            self.out_tokens.append(tok)
        return tok

    def dma_sw(self, key, pairs, r=(), w=()):
        for o, i in pairs:
            tok = self.dma("pool", o, i, r=r, w=w)
        return tok

    def alias(self, news, olds):
        toks = []
        for o in olds:
            if o in self.lastw:
                toks.append(self.lastw[o])
            toks += self.readers.get(o, [])
        for n in news:
            self.readers[n] = self.readers.get(n, []) + toks

    def finish(self, eng="sp"):
        for t in self.out_tokens:
            self._need(eng, t)

    def replay(self, eng, e):
        for item in self.q[eng]:
            if item[0] == "wait":
                key, val = item[1], item[2]
                sem = self.sem[key[1]] if key[0] == "eng" else self.dsem[key[1]]
                e.wait_ge(sem, val)
            elif item[0] == "waitsw":
                e.wait_ge(self.swsem[item[1]], item[2])
            elif item[0] == "clear":
                e.sem_clear(self.swsem[item[1]])
            elif item[0] == "swdma":
                e.dma_start(out=item[1], in_=item[2]).then_inc(self.swsem[item[3]], 16)
            elif item[0] == "op":
                item[1](e).then_inc(self.sem[eng], 1)
            else:
                e.dma_start(out=item[1], in_=item[2]).then_inc(self.dsem[item[3]], 16)


def build(S, E, TG=None, dbg=False, upto=99):
    if TG is None:
        TG = min(2048, S)
    NB = S // 128
    NT = S // 512
    NG = S // TG
    GB = TG // 128
    GT = TG // 512
    assert S % TG == 0 and TG % 512 == 0
    nc = bass.Bass("TRN2", target_bir_lowering=False)

    def din(name, shape, dt=F32):
        return nc.dram_tensor(name, list(shape), dt, kind="ExternalInput").ap()

    x_d = din("x", [S, D])
    cols_d = din("cols", [128, 56])
    bgu_d = din("bgu", [128, E * 16])
    rows_d = din("rows", [128, 4096 + 128 + 256 + E])
    wmod_d = din("w_mod", [D, 6 * D])
    win_d = din("w_in", [D, 5120])
    wchl_d = din("wch_l", [8, 128, 12 * 256])
    wout_d = din("w_out", [D, D])
    wr_d = din("w_router", [D, E])
    wgu_d = din("w_gu", [E, 2, 128, 8 * 1024])
    wdn_d = din("w_dn", [E, 2, 128, 4 * 1024])
    bdn_d = din("b_dn", [E, D])
    cst_d = din("cst", [128, 5 * 128 + 4])
    out_d = nc.dram_tensor("out", [S, D], F32, kind="ExternalOutput").ap()
    x1_d = nc.dram_tensor("x1s", [S, D], F32, kind="Internal").ap()
    dbg_d = {}
    if dbg:
        dbg_d["hT"] = nc.dram_tensor("dbg_hT", [128, KC * S], BF16, kind="ExternalOutput").ap()
        dbg_d["ysb"] = nc.dram_tensor("dbg_ysb", [128, 4 * S], BF16, kind="ExternalOutput").ap()
        dbg_d["ydf"] = nc.dram_tensor("dbg_ydf", [128, 4 * S], BF16, kind="ExternalOutput").ap()
        dbg_d["x1"] = nc.dram_tensor("dbg_x1", [S, D], F32, kind="ExternalOutput").ap()
        dbg_d["cw"] = nc.dram_tensor("dbg_cw", [128, NB * E], F32, kind="ExternalOutput").ap()

    win_v = win_d.rearrange("(kc p) c -> p kc c", p=128)

    es = ExitStack()
    with es:
        sc = Sched(nc, es)

        def finalize():
            sc.finish("sp")
            nc._sched_counts = {k: len(v) for k, v in sc.q.items()}
            with nc.Block() as block:
                @block.tensor
                def _(e):
                    sc.replay("pe", e)

                @block.scalar
                def _(e):
                    sc.replay("act", e)

                @block.vector
                def _(e):
                    sc.replay("dve", e)

                @block.gpsimd
                def _(e):
                    sc.replay("pool", e)

                @block.sync
                def _(e):
                    sc.replay("sp", e)

        def sb(name, shape, dt):
            return es.enter_context(nc.sbuf_tensor("t_" + name, list(shape), dt))

        hT = sb("hT", [128, KC, S], BF16)
        RB = max(8 * S, 2 * GB * D, 32768)
        regB = sb("regB", [128, RB], BF16)
        ysbT = regB[:, 0:4 * S].rearrange("p (c s) -> p c s", c=4)
        ydfT = regB[:, 4 * S:8 * S].rearrange("p (c s) -> p c s", c=4)
        CW = 29 * 1024
        regC = sb("regC", [128, CW], BF16)
        cst = sb("cst", [128, 5 * 128 + 4], F32)
        ident = cst[:, 0:128]
        mask_s = cst[:, 384:512]
        mask_i = cst[:, 512:640]
        wkey = cst[:, 640:644]
        cstb = sb("cstb", [128, 3 * 128], BF16)
        identb = cstb[:, 0:128]
        negU = cstb[:, 128:256]
        negL = cstb[:, 256:384]
        colt = sb("colt", [128, 56], F32)
        modc = sb("modc", [128, 48], F32)
        ggm = sb("ggm", [128, D], F32)
        ggf = sb("ggf", [128, D], F32)
        gsub = sb("gsub", [128, 128], F32)
        small = sb("small", [128, 64], F32)
        brt = sb("brt", [128, E], F32)
        cwt2 = sb("cwt2", [128, NB, E], F32)
        bgu = sb("bgut", [128, E * 16], F32)
        wrt = sb("wrt", [128, KC, E], F32)
        psw = [es.enter_context(nc.psum_tensor(f"psw{i}", [128, 1024], F32)) for i in range(4)]
        ps = [psw[i // 2][:, (i % 2) * 512:(i % 2 + 1) * 512] for i in range(8)]
        PS = [f"ps{i}" for i in range(8)]

        def carve(off_b, shape, dt):
            n = int(np.prod(shape[1:]))
            if dt == F32:
                v = regC[:, off_b // 2: off_b // 2 + 2 * n].bitcast(F32)
            else:
                v = regC[:, off_b // 2: off_b // 2 + n]
            if len(shape) == 3:
                v = v.rearrange("p (a b) -> p a b", a=shape[1])
            return v

        KB = 1024
        lam_neg = small[:, 0:1]

        sc.dma("sp", cst[:], cst_d[:, :], w=["cst"])
        sc.dma("sp", colt[:], cols_d[:, :], w=["colt"])
        sc.dma("sp", bgu[:], bgu_d[:, :], w=["bgu"])
        sc.dma("sp", wrt[:], wr_d.rearrange("(kc p) e -> p kc e", p=128), w=["wrt"])
        rowst = carve(0, [128, 4096 + 128 + 256 + E], F32)
        sc.dma("sp", rowst, rows_d[:, :], w=["rowst"])
        sc.op("dve", lambda e: e.tensor_copy(out=cstb[:], in_=cst[:, 0:384]), r=["cst"], w=["cstb"])
        ones = carve(18 * KB, [128, 128], F32)
        sc.op("dve", lambda e: e.memset(ones, 1.0), w=["ones"])
        sc.op("act", lambda e: e.activation(out=modc[:, 32:40], in_=colt[:, 0:8], func=AF.Silu), r=["colt"], w=["cact"])
        cbc = carve(19 * KB, [128, KC, 128], F32)
        for kc in range(KC):
            sc.op("dve", lambda e, kc=kc: e.tensor_scalar(out=cbc[:, kc, :], in0=ones, scalar1=modc[:, 32 + kc:33 + kc],
                                                         scalar2=None, op0=ALU.mult), r=["cact", "ones"], w=["cbc"])
        wm = [regB[:, i * 16384:(i + 1) * 16384].bitcast(F32).rearrange("p (k c) -> p k c", k=KC) for i in range(2)]
        wmod_v = wmod_d.rearrange("(kc p) c -> p kc c", p=128)
        col_secs = {0: 0, 1: 8, 3: 16, 4: 24}
        for sec in range(6):
            b = sec % 2
            for kc in range(KC):
                sc.dma("sp", wm[b][:, kc, :], wmod_v[:, kc, sec * D:(sec + 1) * D], w=[f"wm{b}_{kc}"])
            if sec in col_secs:
                for j in range(8):
                    for kc in range(KC):
                        sc.op("pe", lambda e, b=b, j=j, kc=kc, sec=sec: e.matmul(
                            ps[7][:, col_secs[sec] + j:col_secs[sec] + j + 1], lhsT=wm[b][:, kc, j * 128:(j + 1) * 128],
                            rhs=modc[:, 32 + kc:33 + kc], start=(kc == 0), stop=(kc == KC - 1), skip_group_check=True),
                            r=[f"wm{b}_{kc}", "cact"], w=[PS[7]])
            else:
                tgt, ro = (ggm, 0) if sec == 2 else (ggf, 1024)
                for hf in range(2):
                    pb = 5 + hf
                    for kc in range(KC):
                        sc.op("pe", lambda e, b=b, kc=kc, hf=hf, pb=pb, sec=sec: e.matmul(
                            ps[pb][:, :], lhsT=cbc[:, kc, :], rhs=wm[b][:, kc, hf * 512:(hf + 1) * 512],
                            start=(kc == 0), stop=(kc == KC - 1)), r=[f"wm{b}_{kc}", "cbc"], w=[PS[pb]])
                    sc.op("dve", lambda e, pb=pb, hf=hf, tgt=tgt, ro=ro: e.tensor_tensor(
                        out=tgt[:, hf * 512:(hf + 1) * 512], in0=ps[pb][:, :], in1=rowst[:, ro + hf * 512:ro + (hf + 1) * 512],
                        op=ALU.add), r=["rowst"], w=[PS[pb], "gg"])
                    sc.op("dve", lambda e, hf=hf, tgt=tgt, ro=ro: e.tensor_tensor(
                        out=tgt[:, hf * 512:(hf + 1) * 512], in0=tgt[:, hf * 512:(hf + 1) * 512],
                        in1=rowst[:, 2048 + ro + hf * 512:2048 + ro + (hf + 1) * 512], op=ALU.mult), r=["rowst"], w=["gg"])
        sc.op("dve", lambda e: e.tensor_tensor(out=modc[:, 0:32], in0=ps[7][:, 0:32], in1=colt[:, 8:40], op=ALU.add),
              r=["colt"], w=[PS[7], "modc"])
        sc.op("dve", lambda e: e.scalar_tensor_tensor(out=modc[:, 8:16], in0=modc[:, 8:16], scalar=1.0, in1=colt[:, 40:48],
                                                      op0=ALU.add, op1=ALU.mult), r=["colt"], w=["modc"])
        sc.op("dve", lambda e: e.scalar_tensor_tensor(out=modc[:, 24:32], in0=modc[:, 24:32], scalar=1.0, in1=colt[:, 48:56],
                                                      op0=ALU.add, op1=ALU.mult), r=["colt"], w=["modc"])
        sc.op("dve", lambda e: e.tensor_scalar(out=gsub[:], in0=rowst[:, 4096:4224], scalar1=float(1.0 - LAM_INIT), scalar2=None,
                                               op0=ALU.mult), r=["rowst"], w=["gsub"])
        sc.op("dve", lambda e: e.tensor_copy(out=brt[:], in_=rowst[:, 4480:4480 + E]), r=["rowst"], w=["brt"])
        lt = carve(23 * KB, [128, 128], F32)
        sc.op("dve", lambda e: e.tensor_tensor(out=lt[:, 0:64], in0=rowst[:, 4224:4288], in1=rowst[:, 4288:4352], op=ALU.mult),
              r=["rowst"], w=["lt"])
        sc.op("dve", lambda e: e.tensor_tensor(out=lt[:, 64:128], in0=rowst[:, 4352:4416], in1=rowst[:, 4416:4480], op=ALU.mult),
              r=["rowst"], w=["lt"])
        sc.op("dve", lambda e: e.reduce_sum(out=small[:, 1:2], in_=lt[:, 0:64], axis=AX.X), r=["lt"], w=["sm1"])
        sc.op("dve", lambda e: e.reduce_sum(out=small[:, 2:3], in_=lt[:, 64:128], axis=AX.X), r=["lt"], w=["sm2"])
        sc.op("act", lambda e: e.activation(out=small[:, 3:5], in_=small[:, 1:3], func=AF.Exp), r=["sm1", "sm2"], w=["sm3"])
        sc.op("dve", lambda e: e.scalar_tensor_tensor(out=lam_neg, in0=small[:, 4:5], scalar=float(-LAM_INIT), in1=small[:, 3:4],
                                                      op0=ALU.add, op1=ALU.subtract), r=["sm3"], w=["lam"])
        bgu3 = bgu[:].rearrange("p (e c) -> p e c", c=16)
        sc.op("dve", lambda e: e.tensor_scalar(out=bgu3[:, :, 8:16], in0=bgu3[:, :, 8:16], scalar1=1.0, scalar2=None, op0=ALU.add),
              r=["bgu"], w=["bgu"])

        if upto == 0:
            finalize()
            return nc
        xt = [carve((24 + 4 * i) * KB, [128, D], F32) for i in range(2)]
        junk = carve(32 * KB, [128, D], BF16)

        def rms_rstd(src_ap, n, dst, src_res, tag, jk, jres="junk"):
            sc.op("act", lambda e: e.activation(out=jk[:, 0:n], in_=src_ap, func=AF.Square, accum_out=dst),
                  r=src_res, w=[jres, tag])
            sc.op("dve", lambda e: e.tensor_scalar(out=dst, in0=dst, scalar1=1.0 / n, scalar2=EPS, op0=ALU.mult, op1=ALU.add),
                  w=[tag])
            sc.op("act", lambda e: e.activation(out=dst, in_=dst, func=AF.Ln), w=[tag])
            sc.op("act", lambda e: e.activation(out=dst, in_=dst, func=AF.Exp, scale=-0.5), w=[tag])

        def norm_transpose(xtile, xres, tb, sh0, gs0, want_f32=None, pbase=0):
            for g4 in range(2):
                pb = pbase + g4
                for j in range(4):
                    kc = g4 * 4 + j
                    sc.op("pe", lambda e, pb=pb, j=j, kc=kc: e.transpose(ps[pb][:, j * 128:(j + 1) * 128], xtile[:, kc * 128:(kc + 1) * 128],
                                                                      ident), r=[xres, "cst"], w=[PS[pb]])
                for j in range(4):
                    kc = g4 * 4 + j
                    sc.op("act", lambda e, pb=pb, j=j, kc=kc: e.activation(
                        out=hT[:, kc, tb * 128:(tb + 1) * 128], in_=ps[pb][:, j * 128:(j + 1) * 128], func=AF.Identity,
                        scale=modc[:, gs0 + kc:gs0 + kc + 1], bias=modc[:, sh0 + kc:sh0 + kc + 1]),
                        r=["modc"], w=[PS[pb], f"hT{tb // 4}"])
                    if want_f32 is not None:
                        sc.op("dve", lambda e, pb=pb, j=j, kc=kc: e.tensor_scalar(
                            out=want_f32[:, kc, :], in0=ps[pb][:, j * 128:(j + 1) * 128], scalar1=modc[:, gs0 + kc:gs0 + kc + 1],
                            scalar2=modc[:, sh0 + kc:sh0 + kc + 1], op0=ALU.mult, op1=ALU.add), r=["modc"], w=[PS[pb], "h2f"])

        for tb in range(NB + 1):
            if tb < NB:
                b = tb % 2
                sc.dma("sp", xt[b], x_d[tb * 128:(tb + 1) * 128, :], w=[f"xt{b}"])
                rms_rstd(xt[b], D, small[:, 8 + b:9 + b], [f"xt{b}"], f"rs{b}", junk)
                sc.op("dve", lambda e, b=b: e.tensor_scalar(out=xt[b], in0=xt[b], scalar1=small[:, 8 + b:9 + b], scalar2=None,
                                                             op0=ALU.mult), r=[f"rs{b}"], w=[f"xt{b}"])
            if tb >= 1:
                pb_ = (tb - 1) % 2
                norm_transpose(xt[pb_], f"xt{pb_}", tb - 1, 0, 8, pbase=2 * pb_)
        if dbg:
            sc.dma("sp", dbg_d["hT"], hT[:].rearrange("p k s -> p (k s)"), r=[f"hT{i}" for i in range(NT)], is_out=True)

        if upto == 1:
            finalize()
            return nc
        qT = carve(0, [128, S], BF16)
        kT = carve(2 * S, [128, S], BF16)
        vt = carve(4 * S, [128, NB, 130], BF16)
        o_w = 4 * S + 260 * NB
        o_w = (o_w + 63) // 64 * 64
        wst = [carve(o_w + 6 * KB * i, [128, KC, 384], BF16) for i in range(2)]
        o_t = o_w + 12 * KB
        e_w = [carve(o_t + p * 4 * KB, [128, 2, 512], F32) for p in range(2)]
        o_t += 8 * KB
        sp_w = [carve(o_t + p * 2 * KB, [128, 2, 512], BF16) for p in range(2)]
        o_t += 4 * KB
        a_w = [carve(o_t + p * 2 * KB, [128, 2, 512], BF16) for p in range(2)]
        o_t += 4 * KB
        g_w = carve(o_t, [128, 2, 512], F32)
        o_t += 4 * KB
        assert o_t <= 2 * CW, o_t
        o_d = o_w + 12 * KB
        p_t = [carve(o_d + i * 512, [128, 256], BF16) for i in range(4)]
        o1_tt = [carve(o_d + 2 * KB + 512 * i, [128, 128], F32) for i in range(2)]
        o2_t = carve(o_d + 3 * KB, [128, 128], F32)
        yb_t = [carve(o_d + 3 * KB + 512 + 256 * i, [128, 128], BF16) for i in range(2)]
        onesb = carve(o_d + 4 * KB, [128, NB], F32)

        WM = [f"wm{i}_{k}" for i in range(2) for k in range(KC)]
        P01 = ["rowst", "ones", "cbc", "lt", "xt0", "xt1", "junk"]
        SBT = ["e0", "e1", "sp0", "sp1", "a0", "a1", "g"]
        sc.alias(["qT", "kT", "vt", "wst0", "wst1"] + SBT, P01)
        sc.alias(["ysbT", "ydfT"], WM)
        wst_n = [0]

        def load_w3(c_q, c_k, c_v):
            b = wst_n[0] % 2
            wst_n[0] += 1
            sc.dma_sw(f"wst{b}", [(wst[b][:, :, i * 128:(i + 1) * 128], win_v[:, :, c0:c0 + 128]) for i, c0 in enumerate((c_q, c_k, c_v))],
                      w=[f"wst{b}"])
            return b

        def project(b, v_scale_col):
            for T in range(NT):
                tok = slice(T * 512, (T + 1) * 512)
                for which, dst, pb in ((0, qT, 0), (1, kT, 1)):
                    for kc in range(KC):
                        sc.op("pe", lambda e, kc=kc, which=which, pb=pb, tok=tok: e.matmul(
                            ps[pb][:, :], lhsT=wst[b][:, kc, which * 128:(which + 1) * 128], rhs=hT[:, kc, tok],
                            start=(kc == 0), stop=(kc == KC - 1)), r=[f"wst{b}", f"hT{T}"], w=[PS[pb]])
                    if which == 0:
                        sc.op("act", lambda e, pb=pb, tok=tok: e.activation(out=qT[:, tok], in_=ps[pb][:, :], func=AF.Copy, scale=0.125),
                              w=[PS[pb], "qT"])
                    else:
                        sc.op("dve", lambda e, pb=pb, tok=tok: e.tensor_copy(out=kT[:, tok], in_=ps[pb][:, :]), w=[PS[pb], "kT"])
                pb = 2 + (T % 2)
                for j in range(4):
                    blk = T * 4 + j
                    for kc in range(KC):
                        sc.op("pe", lambda e, kc=kc, j=j, blk=blk, pb=pb: e.matmul(
                            ps[pb][:, j * 128:(j + 1) * 128], lhsT=hT[:, kc, blk * 128:(blk + 1) * 128], rhs=wst[b][:, kc, 256:384],
                            start=(kc == 0), stop=(kc == KC - 1), skip_group_check=True), r=[f"wst{b}", f"hT{T}"], w=[PS[pb]])
                for j in range(4):
                    blk = T * 4 + j
                    if v_scale_col is None:
                        sc.op("dve", lambda e, j=j, blk=blk, pb=pb: e.tensor_copy(out=vt[:, blk, 0:128], in_=ps[pb][:, j * 128:(j + 1) * 128]),
                              w=[PS[pb], "vt"])
                    else:
                        sc.op("dve", lambda e, j=j, blk=blk, pb=pb: e.tensor_scalar(
                            out=vt[:, blk, 0:128], in0=ps[pb][:, j * 128:(j + 1) * 128], scalar1=v_scale_col, scalar2=None, op0=ALU.mult),
                            r=["cst"], w=[PS[pb], "vt"])

        nb = load_w3(0, 512, 1024)
        for hp in range(4):
            b = nb
            project(b, None)
            if hp < 3:
                nb = load_w3((hp + 1) * 128, 512 + (hp + 1) * 128, 1024 + (hp + 1) * 128)
            else:
                nb = load_w3(1536, 2048, 2560)
            steps = []
            for QT in range(NT):
                nsteps = QT * 4 + 4
                for st in range(nsteps):
                    steps.append((QT, st, nsteps))

            def sb_ctx(i, hd):
                QT, st, nsteps = steps[i]
                kb = nsteps - 1 - st
                c0 = max(0, kb - QT * 4) * 128
                par = i % 2
                return dict(QT=QT, st=st, nsteps=nsteps, kb=kb, c0=c0, cs=slice(c0, 512), par=par, q0=QT * 512,
                            rows=slice(hd * 64, (hd + 1) * 64), zb=par * 2 + hd, rb=4 + hd, ob=6 + (QT % 2), hd=hd)

            def st_A(c):
                sc.op("pe", lambda e, c=c: e.matmul(ps[c["zb"]][:, c["cs"]], lhsT=kT[c["rows"], c["kb"] * 128:(c["kb"] + 1) * 128],
                                                    rhs=qT[c["rows"], c["q0"] + c["c0"]:c["q0"] + 512], start=True, stop=True),
                      r=["qT", "kT"], w=[PS[c["zb"]]])

            def st_B1(c):
                par, cs, c0 = c["par"], c["cs"], c["c0"]
                zw = psw[par].rearrange("p (h c) -> p h c", h=2)
                sc.op("act", lambda e: e.activation(out=e_w[par][:, :, cs], in_=zw[:, :, cs], func=AF.Exp),
                      w=[PS[par * 2], PS[par * 2 + 1], f"e{par}"])

            def st_B2(c):
                par, cs, c0 = c["par"], c["cs"], c["c0"]
                if c["kb"] >= c["QT"] * 4:
                    for hd in range(2):
                        sc.op("pool", lambda e, hd=hd: e.tensor_tensor(out=e_w[par][:, hd, c0:c0 + 128], in0=e_w[par][:, hd, c0:c0 + 128],
                                                                    in1=mask_s, op=ALU.mult), r=["cst"], w=[f"e{par}"])
                sc.op("act", lambda e: e.activation(out=sp_w[par][:, :, cs], in_=e_w[par][:, :, cs], func=AF.Ln, bias=1.0),
                      r=[f"e{par}"], w=[f"sp{par}"])

            def st_C1(c):
                par, cs = c["par"], c["cs"]
                sc.op("pe", lambda e, c=c: e.matmul(ps[c["rb"]][:, cs], lhsT=negU, rhs=sp_w[par][:, c["hd"], cs], start=(c["st"] == 0), stop=False,
                                                    skip_group_check=True), r=[f"sp{par}", "cstb"], w=[PS[c["rb"]]])

            def st_C2(c):
                cs = c["cs"]
                rw = psw[2].rearrange("p (h c) -> p h c", h=2)
                sc.op("act", lambda e: e.activation(out=g_w[:, :, cs], in_=rw[:, :, cs], func=AF.Exp), w=[PS[4], PS[5], "g"])

            def st_C3(c):
                par, cs = c["par"], c["cs"]
                if c["kb"] > 0:
                    sc.op("pe", lambda e, c=c: e.matmul(ps[c["rb"]][:, cs], lhsT=negL, rhs=sp_w[par][:, c["hd"], cs], start=False, stop=False,
                                                        skip_group_check=True), r=[f"sp{par}", "cstb"], w=[PS[c["rb"]]])

            def st_D(c):
                par, cs = c["par"], c["cs"]
                sc.op("dve", lambda e: e.tensor_tensor(out=a_w[par][:, :, cs], in0=e_w[par][:, :, cs], in1=g_w[:, :, cs], op=ALU.mult),
                      r=[f"e{par}", "g"], w=[f"a{par}"])

            def st_E(c):
                par, cs = c["par"], c["cs"]
                sc.op("pe", lambda e, c=c: e.matmul(ps[c["ob"]][c["rows"], cs], lhsT=vt[:, c["kb"], c["rows"]], rhs=a_w[par][:, c["hd"], cs],
                                                    start=(c["st"] == 0), stop=(c["st"] == c["nsteps"] - 1), skip_group_check=True),
                      r=[f"a{par}", "vt"], w=[PS[c["ob"]]])

            n = len(steps)
            for it in range(n + 3):
                if 0 <= it - 2 < n:
                    for hd in range(2):
                        st_C1(sb_ctx(it - 2, hd))
                if 0 <= it - 1 < n:
                    st_B1(sb_ctx(it - 1, 0))
                if 0 <= it - 2 < n:
                    st_C2(sb_ctx(it - 2, 0))
                if it < n:
                    for hd in range(2):
                        st_A(sb_ctx(it, hd))
                if 0 <= it - 3 < n:
                    for hd in range(2):
                        st_E(sb_ctx(it - 3, hd))
                    c = sb_ctx(it - 3, 0)
                    if c["st"] == c["nsteps"] - 1:
                        sc.op("dve", lambda e, c=c, hp=hp: e.tensor_copy(out=ysbT[:, hp, c["q0"]:c["q0"] + 512], in_=ps[c["ob"]][:, :]),
                              w=[PS[c["ob"]], "ysbT"])
                if 0 <= it - 2 < n:
                    for hd in range(2):
                        st_C3(sb_ctx(it - 2, hd))
                    st_D(sb_ctx(it - 2, 0))
                if 0 <= it - 1 < n:
                    st_B2(sb_ctx(it - 1, 0))
        if dbg:
            sc.dma("sp", dbg_d["ysb"], regB[:, 0:4 * S], r=["ysbT"], is_out=True)

        if upto == 2:
            finalize()
            return nc
        sc.alias(["p0", "p1", "p2", "p3", "o10", "o11", "o2", "yb0", "yb1", "onesb"], SBT)
        sc.op("dve", lambda e: e.memset(onesb, 1.0), w=["onesb"])
        for h in range(4):
            b = nb
            slope = 2.0 ** (-2.0 * (h + 1))
            project(b, wkey[:, h:h + 1])
            sc.op("dve", lambda e, h=h: e.tensor_scalar(out=vt[:, :, 128], in0=onesb, scalar1=wkey[:, h:h + 1], scalar2=None, op0=ALU.mult),
                  r=["onesb", "cst"], w=["vt"])
            if h < 3:
                nb = load_w3(1536 + (h + 1) * 128, 2048 + (h + 1) * 128, 2560 + (h + 1) * 128)
            dsteps = [(qb, st) for qb in range(NB) for st in range(qb + 1)]

            def df_ctx(i):
                qb, st = dsteps[i]
                kb = qb - st
                return dict(qb=qb, st=st, kb=kb, zb=i % 2, pt=p_t[i % 4], ptag=f"p{i % 4}", ob=4 + (qb % 2),
                            qs=slice(qb * 128, (qb + 1) * 128), bias=float(-slope * 128.0 * (qb - kb)))

            def df_A(c):
                for m in range(2):
                    rows = slice(m * 64, (m + 1) * 64)
                    sc.op("pe", lambda e, c=c, m=m, rows=rows: e.matmul(ps[2 * c["zb"] + m][:, 0:128], lhsT=kT[rows, c["kb"] * 128:(c["kb"] + 1) * 128],
                                                                     rhs=qT[rows, c["qs"]], start=True, stop=True),
                          r=["qT", "kT"], w=[PS[2 * c["zb"] + m]])

            def df_B(c):
                zw = psw[c["zb"]].rearrange("p (m c) -> p m c", m=2)
                ptw = c["pt"].rearrange("p (m c) -> p m c", m=2)
                sc.op("act", lambda e, c=c: e.activation(out=ptw, in_=zw[:, :, 0:128], func=AF.Exp, bias=c["bias"]),
                      w=[PS[2 * c["zb"]], PS[2 * c["zb"] + 1], c["ptag"]])
                if c["kb"] == c["qb"]:
                    for m in range(2):
                        sc.op("pool", lambda e, c=c, m=m: e.tensor_tensor(out=c["pt"][:, m * 128:(m + 1) * 128], in0=c["pt"][:, m * 128:(m + 1) * 128],
                                                                       in1=mask_i, op=ALU.mult), r=["cst"], w=[c["ptag"]])

            def df_C(c):
                for m in range(2):
                    sc.op("pe", lambda e, c=c, m=m: e.matmul(ps[c["ob"]][:, m * 256:m * 256 + 129], lhsT=c["pt"][:, m * 128:(m + 1) * 128],
                                                             rhs=vt[:, c["kb"], 0:129], start=(c["st"] == 0 and m == 0), stop=(c["st"] == c["qb"]),
                                                             skip_group_check=True), r=[c["ptag"], "vt"], w=[PS[c["ob"]]])

            def df_epi1(c):
                ob = c["ob"]
                o1_t = o1_tt[c["qb"] % 2]
                r3 = small[:, 18 + (c["qb"] % 2):19 + (c["qb"] % 2)]
                o1n = f"o1{c['qb'] % 2}"
                r3n = f"r3{c['qb'] % 2}"
                sc.op("dve", lambda e: e.reciprocal(out=small[:, 16:17], in_=ps[ob][:, 128:129]), w=[PS[ob], "r1"])
                sc.op("dve", lambda e: e.reciprocal(out=small[:, 17:18], in_=ps[ob][:, 384:385]), w=[PS[ob], "r2"])
                sc.op("dve", lambda e: e.tensor_tensor(out=small[:, 17:18], in0=small[:, 17:18], in1=lam_neg, op=ALU.mult), r=["lam"], w=["r2"])
                sc.op("dve", lambda e: e.tensor_scalar(out=o1_t, in0=ps[ob][:, 0:128], scalar1=small[:, 16:17], scalar2=None, op0=ALU.mult),
                      r=["r1"], w=[PS[ob], o1n])
                sc.op("dve", lambda e: e.scalar_tensor_tensor(out=o1_t, in0=ps[ob][:, 256:384], scalar=small[:, 17:18], in1=o1_t,
                                                              op0=ALU.mult, op1=ALU.add), r=["r2"], w=[PS[ob], o1n])
                sc.op("dve", lambda e: e.tensor_tensor(out=o2_t, in0=o1_t, in1=o1_t, op=ALU.mult), r=[o1n], w=["o2"])
                sc.op("dve", lambda e: e.reduce_sum(out=r3, in_=o2_t, axis=AX.X), r=["o2"], w=[r3n])
                sc.op("dve", lambda e: e.tensor_scalar(out=r3, in0=r3, scalar1=1.0 / 128, scalar2=EPS,
                                                       op0=ALU.mult, op1=ALU.add), w=[r3n])

            def df_epi2(c):
                qb = c["qb"]
                o1_t = o1_tt[qb % 2]
                r3 = small[:, 18 + (qb % 2):19 + (qb % 2)]
                o1n = f"o1{qb % 2}"
                r3n = f"r3{qb % 2}"
                sc.op("act", lambda e: e.activation(out=r3, in_=r3, func=AF.Ln), w=[r3n])
                sc.op("act", lambda e: e.activation(out=r3, in_=r3, func=AF.Exp, scale=-0.5), w=[r3n])
                yb = yb_t[qb % 2]
                sc.op("dve", lambda e: e.scalar_tensor_tensor(out=yb, in0=o1_t, scalar=r3, in1=gsub[:],
                                                              op0=ALU.mult, op1=ALU.mult), r=[o1n, r3n, "gsub"], w=[f"yb{qb % 2}"])
                tb_ = 6 + (qb % 2)
                psb = ps[tb_][:, 0:256].bitcast(BF16)
                sc.op("pe", lambda e: e.transpose(psb[:, 0:128], yb, identb), r=[f"yb{qb % 2}", "cstb"], w=[PS[tb_]])
                sc.op("dve", lambda e, h=h: e.tensor_copy(out=ydfT[:, h, c["qs"]], in_=psb[:, 0:128]), w=[PS[tb_], "ydfT"])

            n = len(dsteps)
            pend = []
            for it in range(n + 6):
                if it < n:
                    df_A(df_ctx(it))
                if 0 <= it - 1 < n:
                    df_B(df_ctx(it - 1))
                if 0 <= it - 2 < n:
                    c = df_ctx(it - 2)
                    df_C(c)
                    if c["st"] == c["qb"]:
                        while pend and pend[0][1]["qb"] <= c["qb"] - 2:
                            df_epi2(pend.pop(0)[1])
                        df_epi1(c)
                        pend.append((it + 3, c))
                while pend and pend[0][0] <= it:
                    df_epi2(pend.pop(0)[1])
            assert not pend
        if dbg:
            sc.dma("sp", dbg_d["ydf"], regB[:, 4 * S:8 * S], r=["ydfT"], is_out=True)

        if upto == 3:
            finalize()
            return nc
        wbr = carve(0, [128, 8, 1024], BF16)
        wch2d = [carve(16 * KB + 6 * KB * i, [128, 12 * 256], BF16) for i in range(2)]
        wch = [w2.rearrange("p (a b) -> p a b", a=12) for w2 in wch2d]
        mrg = carve(28 * KB, [128, 8, 512], BF16)
        s1 = carve(36 * KB, [128, 512], F32)
        s2 = carve(38 * KB, [128, 512], F32)
        m1 = carve(40 * KB, [128, 512], F32)
        xt3 = [carve((42 + 4 * i) * KB, [128, D], F32) for i in range(2)]
        junk3 = carve(50 * KB, [128, D], BF16)
        h2f = carve(52 * KB, [128, KC, 128], F32)
        P3 = ["qT", "kT", "vt", "wst0", "wst1", "e0", "e1", "sp0", "sp1", "a0", "a1", "g", "p0", "p1", "p2", "p3", "o10", "o11", "o2", "yb0", "yb1", "onesb"]
        sc.alias(["wbr", "wch0", "wch1", "mrg", "s1", "s2", "m1", "x30", "x31", "junk", "h2f"], P3)
        wout_v = wout_d.rearrange("(kc p) c -> p kc c", p=128)
        sc.dma_sw("wbr", [(wbr[:, kc, :], wout_v[:, kc, :]) for kc in range(KC)], w=["wbr"])
        wch_n = [0]

        def load_wch(fc):
            b = wch_n[0] % 2
            wch_n[0] += 1
            sc.dma_sw(f"wch{b}", [(wch2d[b], wchl_d[fc])], w=[f"wch{b}"])
            return b

        nbw = load_wch(0)
        for T in range(NT):
            tok = slice(T * 512, (T + 1) * 512)
            for fc in range(8):
                b = nbw
                nxt = (T * 8 + fc + 1)
                if nxt < NT * 8:
                    nbw = load_wch(nxt % 8)
                s4 = 4 * (fc % 2)
                for gi, pb in ((0, s4), (1, s4 + 1)):
                    for kc in range(KC):
                        sc.op("pe", lambda e, b=b, gi=gi, pb=pb, kc=kc, tok=tok: e.matmul(
                            ps[pb][:, :], lhsT=wch[b][:, kc, gi * 128:(gi + 1) * 128], rhs=hT[:, kc, tok], start=(kc == 0), stop=(kc == KC - 1)),
                            r=[f"wch{b}", f"hT{T}"], w=[PS[pb]])
                for bi, pb, ysrc, yres in ((0, s4 + 2, ysbT, "ysbT"), (1, s4 + 3, ydfT, "ydfT")):
                    for c in range(4):
                        sc.op("pe", lambda e, b=b, bi=bi, pb=pb, c=c, ysrc=ysrc, tok=tok: e.matmul(
                            ps[pb][:, :], lhsT=wch[b][:, 8 + c, bi * 128:(bi + 1) * 128], rhs=ysrc[:, c, tok], start=(c == 0), stop=(c == 3)),
                            r=[f"wch{b}", yres], w=[PS[pb]])
                sc.op("act", lambda e, s4=s4: e.activation(out=s1, in_=ps[s4][:, :], func=AF.Sigmoid), w=[PS[s4], "s1"])
                sc.op("act", lambda e, s4=s4: e.activation(out=s2, in_=ps[s4 + 1][:, :], func=AF.Sigmoid), w=[PS[s4 + 1], "s2"])
                sc.op("dve", lambda e, s4=s4: e.tensor_tensor(out=m1, in0=s1, in1=ps[s4 + 2][:, :], op=ALU.mult), r=["s1"], w=[PS[s4 + 2], "m1"])
                sc.op("dve", lambda e, s4=s4: e.tensor_tensor(out=s2, in0=s2, in1=ps[s4 + 3][:, :], op=ALU.mult), w=[PS[s4 + 3], "s2"])
                sc.op("pool", lambda e, fc=fc: e.tensor_tensor(out=mrg[:, fc, :], in0=m1, in1=s2, op=ALU.add), r=["m1", "s2"], w=["mrg"])
            def Q1(j):
                tb = T * 4 + j
                b2 = tb % 2
                ob0 = 4 * (j % 2)
                ss0 = 20 if j % 2 == 0 else 28
                sc.dma("sp", xt3[b2], x_d[tb * 128:(tb + 1) * 128, :], w=[f"x3{b2}"])
                for hf in range(2):
                    pb = ob0 + hf
                    for fc in range(8):
                        sc.op("pe", lambda e, pb=pb, fc=fc, hf=hf: e.matmul(
                            ps[pb][:, :], lhsT=mrg[:, fc, j * 128:(j + 1) * 128], rhs=wbr[:, fc, hf * 512:(hf + 1) * 512], start=(fc == 0),
                            stop=(fc == 7)), r=["mrg", "wbr"], w=[PS[pb]])
                    sc.op("act", lambda e, pb=pb, hf=hf: e.activation(out=junk3[:, 0:512], in_=ps[pb][:, :], func=AF.Square,
                                                                   accum_out=small[:, ss0 + hf:ss0 + hf + 1]), w=[PS[pb], "junk", f"ssm{j % 2}{hf}"])

            def Q2(j):
                tb = T * 4 + j
                b2 = tb % 2
                ob0 = 4 * (j % 2)
                ss0 = 20 if j % 2 == 0 else 28
                sc.op("dve", lambda e: e.tensor_tensor(out=small[:, 22:23], in0=small[:, ss0:ss0 + 1], in1=small[:, ss0 + 1:ss0 + 2], op=ALU.add),
                      r=[f"ssm{j % 2}0", f"ssm{j % 2}1"], w=["rsm"])
                sc.op("dve", lambda e: e.tensor_scalar(out=small[:, 22:23], in0=small[:, 22:23], scalar1=1.0 / D, scalar2=EPS,
                                                       op0=ALU.mult, op1=ALU.add), w=["rsm"])
                sc.op("act", lambda e: e.activation(out=small[:, 22:23], in_=small[:, 22:23], func=AF.Ln), w=["rsm"])
                sc.op("act", lambda e: e.activation(out=small[:, 22:23], in_=small[:, 22:23], func=AF.Exp, scale=-0.5), w=["rsm"])
                for hf in range(2):
                    pb = ob0 + hf
                    hs = slice(hf * 512, (hf + 1) * 512)
                    sc.op("dve", lambda e, pb=pb, hs=hs: e.scalar_tensor_tensor(out=m1, in0=ps[pb][:, :], scalar=small[:, 22:23], in1=ggm[:, hs],
                                                                             op0=ALU.mult, op1=ALU.mult), r=["rsm", "gg"], w=[PS[pb], "m1"])
                    sc.op("pool", lambda e, hs=hs, b2=b2: e.tensor_tensor(out=xt3[b2][:, hs], in0=xt3[b2][:, hs], in1=m1, op=ALU.add),
                          r=["m1"], w=[f"x3{b2}"])
                sc.dma("sp", x1_d[tb * 128:(tb + 1) * 128, :], xt3[b2], r=[f"x3{b2}"], w=["x1d"])
                if dbg:
                    sc.dma("sp", dbg_d["x1"][tb * 128:(tb + 1) * 128, :], xt3[b2], r=[f"x3{b2}"], is_out=True)
                rms_rstd(xt3[b2], D, small[:, 24:25], [f"x3{b2}"], "rs3", junk3)
                sc.op("dve", lambda e, b2=b2: e.tensor_scalar(out=xt3[b2], in0=xt3[b2], scalar1=small[:, 24:25], scalar2=None, op0=ALU.mult),
                      r=["rs3"], w=[f"x3{b2}"])
                norm_transpose(xt3[b2], f"x3{b2}", tb, 16, 24, want_f32=h2f, pbase=2)
                for kc in range(KC):
                    sc.op("pe", lambda e, kc=kc: e.matmul(ps[6][:, 0:E], lhsT=h2f[:, kc, :], rhs=wrt[:, kc, :], start=(kc == 0),
                                                          stop=(kc == KC - 1)), r=["h2f", "wrt"], w=[PS[6]])
                lg = small[:, 32:32 + E]
                sc.op("dve", lambda e: e.tensor_tensor(out=lg, in0=ps[6][:, 0:E], in1=brt[:], op=ALU.add), r=["brt"], w=[PS[6], "lg"])
                sc.op("dve", lambda e: e.max(out=modc[:, 40:48], in_=lg), r=["lg"], w=["mx8"])
                sc.op("dve", lambda e: e.tensor_scalar(out=small[:, 25:26], in0=modc[:, 40:41], scalar1=-1.0, scalar2=None, op0=ALU.mult),
                      r=["mx8"], w=["nmx"])
                ex = m1[:, 0:E]
                sc.op("act", lambda e: e.activation(out=ex, in_=lg, func=AF.Exp, bias=small[:, 25:26]), r=["lg", "nmx"], w=["m1"])
                sc.op("dve", lambda e, tb=tb: e.scalar_tensor_tensor(out=cwt2[:, tb, :], in0=lg, scalar=modc[:, 43:44], in1=ex, op0=ALU.is_ge,
                                                                   op1=ALU.mult), r=["lg", "mx8", "m1"], w=["cwt2"])
                sc.op("dve", lambda e, tb=tb: e.reduce_sum(out=small[:, 26:27], in_=cwt2[:, tb, :], axis=AX.X), r=["cwt2"], w=["den"])
                sc.op("dve", lambda e: e.reciprocal(out=small[:, 26:27], in_=small[:, 26:27]), w=["den"])
                sc.op("dve", lambda e, tb=tb: e.tensor_scalar(out=cwt2[:, tb, :], in0=cwt2[:, tb, :], scalar1=small[:, 26:27],
                                                              scalar2=float(1.0 / 1.702), op0=ALU.mult, op1=ALU.mult), r=["den"], w=["cwt2"])
            Q1(0)
            for j in range(4):
                if j < 3:
                    Q1(j + 1)
                Q2(j)
        if dbg:
            sc.dma("sp", dbg_d["cw"], cwt2[:].rearrange("p a b -> p (a b)"), r=["cwt2"], is_out=True)

        if upto == 4:
            finalize()
            return nc
        acc = regB[:, 0:2 * GB * D].bitcast(F32).rearrange("p (a b) -> p a b", a=GB)
        wgu2d = [carve(16 * KB * i, [128, KC * 1024], BF16) for i in range(2)]
        wgu = [w2.rearrange("p (a b) -> p a b", a=KC) for w2 in wgu2d]
        wdn2d = [carve(32 * KB, [128, 4 * 1024], BF16)]
        wdn = [w2.rearrange("p (a b) -> p a b", a=4) for w2 in wdn2d]
        actT = [carve(40 * KB + 4 * KB * i, [128, 4, 512], BF16) for i in range(2)]
        gtm = [carve(48 * KB + 2 * KB * i, [128, 512], F32) for i in range(2)]
        atm = [carve(52 * KB, [128, 512], F32)] * 2
        ctm = [carve(54 * KB + 2 * KB * i, [128, 512], F32) for i in range(2)]
        x1t = carve(48 * KB, [128, D], F32)
        junk4 = carve(52 * KB, [128, D], BF16)
        cwT = carve(52 * KB, [128, 128], F32)
        bdn = carve(54 * KB, [128, D], F32)
        assert 58 * KB <= 2 * CW
        P4 = ["wbr", "wch0", "wch1", "mrg", "s1", "s2", "m1", "x30", "x31", "h2f", "junk", "ysbT", "ydfT"] + WM
        sc.alias(["wgu0", "wgu1", "wdn0", "actT0", "actT1", "gt0", "gt1", "at0", "ct0", "ct1", "junk"] + [f"acc{j}" for j in range(GB)], P4)
        def load_wgu(e_, hf, b):
            sc.dma_sw(f"wgu{b}", [(wgu2d[b][:, q * 2048:(q + 1) * 2048], wgu_d[e_, hf, :, q * 2048:(q + 1) * 2048]) for q in range(4)],
                      w=[f"wgu{b}"])

        def load_wdn(e_, hf):
            sc.dma_sw("wdn0", [(wdn2d[0][:, q * 2048:(q + 1) * 2048], wdn_d[e_, hf, :, q * 2048:(q + 1) * 2048]) for q in range(2)],
                      w=["wdn0"])

        units = [(g, e_, hf) for g in range(NG) for e_ in range(E) for hf in range(2)]
        load_wgu(0, 0, 0)
        ucount = 0
        acount = 0
        for g in range(NG):
            sc.dma("sp", bdn[0:E, :], bdn_d[:, :], w=["ct0", "ct1"])
            sc.op("act", lambda e: e.activation(out=bdn[0:E, :], in_=bdn[0:E, :], func=AF.Copy, scale=1.702), w=["ct0", "ct1"])
            for jb in range(GB):
                tb = g * GB + jb
                sc.op("pe", lambda e, tb=tb: e.transpose(ps[6][0:E, 0:128], cwt2[:, tb, :], ident), r=["cwt2", "cst"], w=[PS[6]])
                sc.op("dve", lambda e: e.tensor_copy(out=cwT[0:E, :], in_=ps[6][0:E, 0:128]), w=[PS[6], "at0"])
                for hf in range(2):
                    sc.op("pe", lambda e, hf=hf: e.matmul(ps[7][:, :], lhsT=cwT[0:E, :], rhs=bdn[0:E, hf * 512:(hf + 1) * 512], start=True,
                                                          stop=True), r=["at0", "ct0", "ct1"], w=[PS[7]])
                    sc.op("act", lambda e, jb=jb, hf=hf: e.activation(out=acc[:, jb, hf * 512:(hf + 1) * 512], in_=ps[7][:, :], func=AF.Copy),
                          w=[PS[7], f"acc{jb}"])
            def moe_G(u):
                e_, hf, Tl, b, ab = u
                T = g * GT + Tl
                tok = slice(T * 512, (T + 1) * 512)
                for fcl in range(4):
                    pg = (fcl % 2) * 2
                    pu = pg + 1
                    for kc in range(KC):
                        sc.op("pe", lambda e, kc=kc, fcl=fcl, pg=pg: e.matmul(
                            ps[pg][:, :], lhsT=wgu[b][:, kc, fcl * 128:(fcl + 1) * 128], rhs=hT[:, kc, tok], start=(kc == 0),
                            stop=(kc == KC - 1)), r=[f"wgu{b}", f"hT{T}"], w=[PS[pg]])
                    for kc in range(KC):
                        sc.op("pe", lambda e, kc=kc, fcl=fcl, pu=pu: e.matmul(
                            ps[pu][:, :], lhsT=wgu[b][:, kc, 512 + fcl * 128:512 + (fcl + 1) * 128], rhs=hT[:, kc, tok], start=(kc == 0),
                            stop=(kc == KC - 1)), r=[f"wgu{b}", f"hT{T}"], w=[PS[pu]])
                    tp = fcl % 2
                    cg = e_ * 16 + hf * 4 + fcl
                    cu = e_ * 16 + 8 + hf * 4 + fcl
                    sc.op("dve", lambda e, pg=pg, tp=tp, cg=cg: e.tensor_scalar(out=gtm[tp], in0=ps[pg][:, :], scalar1=bgu[:, cg:cg + 1],
                                                                               scalar2=7.0, op0=ALU.add, op1=ALU.min),
                          r=["bgu"], w=[PS[pg], f"gt{tp}"])
                    sc.op("act", lambda e, tp=tp: e.activation(out=gtm[tp], in_=gtm[tp], func=AF.Silu, scale=1.702), w=[f"gt{tp}"])
                    sc.op("dve", lambda e, pu=pu, tp=tp, cu=cu: e.tensor_scalar(out=atm[tp], in0=ps[pu][:, :], scalar1=bgu[:, cu:cu + 1],
                                                                               scalar2=-6.0, op0=ALU.add, op1=ALU.max),
                          r=["bgu"], w=[PS[pu], "at0"])
                    sc.op("dve", lambda e, tp=tp, fcl=fcl: e.scalar_tensor_tensor(out=actT[ab][:, fcl, :], in0=atm[tp], scalar=8.0,
                                                                                in1=gtm[tp], op0=ALU.min, op1=ALU.mult),
                          r=["at0", f"gt{tp}"], w=[f"actT{ab}"])

            def moe_D(u):
                e_, hf, Tl, b, ab = u
                for sub in range(4):
                    jb = Tl * 4 + sub
                    tb = g * GB + jb
                    for h2 in range(2):
                        pd = 4 + h2
                        cp = (sub * 2 + h2) % 2
                        for fcl in range(4):
                            sc.op("pe", lambda e, fcl=fcl, sub=sub, h2=h2, pd=pd: e.matmul(
                                ps[pd][:, :], lhsT=actT[ab][:, fcl, sub * 128:(sub + 1) * 128], rhs=wdn[0][:, fcl, h2 * 512:(h2 + 1) * 512],
                                start=(fcl == 0), stop=(fcl == 3)), r=[f"actT{ab}", "wdn0"], w=[PS[pd]])
                        if h2 == 0:
                            sc.op("dve", lambda e, pd=pd, jb=jb, tb=tb: e.scalar_tensor_tensor(
                                out=acc[:, jb, 0:512], in0=ps[pd][:, :], scalar=cwt2[:, tb, e_:e_ + 1], in1=acc[:, jb, 0:512],
                                op0=ALU.mult, op1=ALU.add), r=["cwt2"], w=[PS[pd], f"acc{jb}"])
                        else:
                            cp = sub % 2
                            sc.op("act", lambda e, pd=pd, cp=cp, tb=tb: e.activation(out=ctm[cp], in_=ps[pd][:, :], func=AF.Copy,
                                                                                 scale=cwt2[:, tb, e_:e_ + 1]),
                                  r=["cwt2"], w=[PS[pd], f"ct{cp}"])
                            sc.op("pool", lambda e, cp=cp, jb=jb: e.tensor_tensor(out=acc[:, jb, 512:1024], in0=acc[:, jb, 512:1024], in1=ctm[cp],
                                                                               op=ALU.add), r=[f"ct{cp}"], w=[f"acc{jb}"])

            gunits = []
            for e_ in range(E):
                for hf in range(2):
                    b = ucount % 2
                    ucount += 1
                    nxt = units[ucount][1:] if ucount < len(units) else None
                    for Tl in range(GT):
                        gunits.append(((e_, hf, Tl, b, acount % 2), nxt if Tl == 0 else None))
                        acount += 1
            prev = None
            for u, nxt in gunits:
                e_, hf, Tl, b, ab = u
                if Tl == 0:
                    if prev is not None:
                        moe_D(prev)
                    load_wdn(e_, hf)
                    if nxt is not None:
                        load_wgu(nxt[0], nxt[1], 1 - b)
                    moe_G(u)
                else:
                    moe_G(u)
                    moe_D(prev)
                prev = u
            moe_D(prev)
            for jb in range(GB):
                tb = g * GB + jb
                b2 = tb % 2
                sc.dma("sp", x1t, x1_d[tb * 128:(tb + 1) * 128, :], r=["x1d"], w=["gt0", "gt1"])
                rms_rstd(acc[:, jb, :], D, small[:, 27:28], [f"acc{jb}"], "rs4", junk4, jres="at0")
                for hf in range(2):
                    hs = slice(hf * 512, (hf + 1) * 512)
                    sc.op("dve", lambda e, jb=jb, hs=hs: e.scalar_tensor_tensor(out=acc[:, jb, hs], in0=acc[:, jb, hs], scalar=small[:, 27:28],
                                                                             in1=ggf[:, hs], op0=ALU.mult, op1=ALU.mult),
                          r=["rs4", "gg"], w=[f"acc{jb}"])
                    sc.op("pool", lambda e, jb=jb, hs=hs: e.tensor_tensor(out=x1t[:, hs], in0=x1t[:, hs], in1=acc[:, jb, hs], op=ALU.add),
                          r=[f"acc{jb}"], w=["gt0", "gt1"])
                sc.dma("sp", out_d[tb * 128:(tb + 1) * 128, :], x1t, r=["gt0", "gt1"], is_out=True)
        finalize()
    return nc


def host_consts():
    c = np.zeros((128, 5 * 128 + 4), np.float32)
    i = np.arange(128)
    c[:, 0:128] = np.eye(128, dtype=np.float32)
    c[:, 128:256] = -(i[:, None] >= i[None, :]).astype(np.float32)
    c[:, 256:384] = -(i[:, None] < i[None, :]).astype(np.float32)
    c[:, 384:512] = (i[:, None] < i[None, :]).astype(np.float32)
    c[:, 512:640] = (i[:, None] <= i[None, :]).astype(np.float32)
    for h in range(4):
        slope = 2.0 ** (-2.0 * (h + 1))
        c[:, 640 + h] = np.exp(-slope * (127 - i)).astype(np.float32)
    return c


def col_layout(v):
    return np.ascontiguousarray(np.asarray(v, np.float32).reshape(-1, 128).T)


def make_in_maps(inputs, S, E, n_cores):
    f = lambda a: np.ascontiguousarray(np.asarray(a, np.float32))
    b_mod = f(inputs["b_mod"])[0]
    secs = [b_mod[i * D:(i + 1) * D] for i in range(6)]
    cst = host_consts()
    rows1 = np.concatenate([secs[2], secs[5], f(inputs["g_post_mix"])[0], f(inputs["g_post_ffn"])[0], f(inputs["g_subln"])[0],
                            f(inputs["lambda_q1"])[0], f(inputs["lambda_k1"])[0], f(inputs["lambda_q2"])[0], f(inputs["lambda_k2"])[0],
                            f(inputs["b_router"])[0]])
    rows = np.ascontiguousarray(np.broadcast_to(rows1[None, :], (128, rows1.size)))
    bgu = np.concatenate([col_layout(f(inputs["b_gate_up"])[0, e]) for e in range(E)], axis=1)
    w_in0 = f(inputs["w_in"])[0]
    wg = w_in0[:, 3072:5120].reshape(8, 128, 2, 8, 128).transpose(3, 1, 0, 2, 4).reshape(8, 128, 8 * 256)
    wb = np.stack([f(inputs["w_branch_sb"])[0], f(inputs["w_branch_diff"])[0]], axis=0).reshape(2, 4, 128, 8, 128)
    wb = wb.transpose(3, 2, 1, 0, 4).reshape(8, 128, 4 * 256)
    wch_l = np.ascontiguousarray(np.concatenate([wg, wb], axis=2))
    wgu_l = np.ascontiguousarray(f(inputs["w_gate_up"])[0].reshape(E, 8, 128, 2, 2, 512).transpose(0, 4, 2, 1, 3, 5)).reshape(E, 2, 128, 8192)
    wdn_l = np.ascontiguousarray(f(inputs["w_down"])[0].reshape(E, 2, 4, 128, 1024).transpose(0, 1, 3, 2, 4)).reshape(E, 2, 128, 4096)
    shared = {
        "bgu": np.ascontiguousarray(bgu), "rows": rows, "w_mod": f(inputs["w_mod"])[0], "w_in": f(inputs["w_in"])[0],
        "wch_l": wch_l, "w_out": f(inputs["w_out"])[0],
        "w_router": f(inputs["w_router"])[0], "w_gu": wgu_l, "w_dn": wdn_l,
        "b_dn": f(inputs["b_down"])[0], "cst": cst,
    }
    x = f(inputs["x"])
    c = f(inputs["c"])
    maps = []
    for b in range(n_cores):
        cols = np.concatenate([col_layout(c[b]), col_layout(secs[0]), col_layout(secs[1]), col_layout(secs[3]), col_layout(secs[4]),
                               col_layout(f(inputs["g_pre_mix"])[0]), col_layout(f(inputs["g_pre_ffn"])[0])], axis=1)
        m = dict(shared)
        m["x"] = np.ascontiguousarray(x[b])
        m["cols"] = np.ascontiguousarray(cols)
        maps.append(m)
    return maps


_NC_CACHE = {}


def kernel(**inputs):
    x = np.asarray(inputs["x"])
    B, S, _ = x.shape
    E = np.asarray(inputs["w_gate_up"]).shape[1]
    key = (S, E)
    if key not in _NC_CACHE:
        _NC_CACHE[key] = build(S, E)
    nc = _NC_CACHE[key]
    maps = make_in_maps(inputs, S, E, B)
    res = run_bass_kernel_spmd(nc, maps, core_ids=list(range(B)))
    return np.stack([np.asarray(r["out"], np.float32) for r in res.results], axis=0)
```
